# Optimizing a Trainium2 kernel written in Bass

```python
import math
import jax
import jax.numpy as jnp
from jax import lax
import numpy as np

D_MODEL = 2048
BATCH = 16
SEQ = 256
DEPTH = 4
DEC_BATCH = 2
DEC_SEQ = 1024
PAST_LEN = 512

GRID_W = 64
ROPE_THETA = 10000.0
Q_BLOCK = 128
N_EVEN = (DEPTH + 1) // 2
N_ODD = DEPTH // 2
DEEPNORM_ALPHA = (2.0 * DEPTH) ** 0.25
DEEPNORM_BETA = (8.0 * DEPTH) ** -0.25
LN_EPS = 1e-5
RMS_EPS = 1e-6

MLA_HEADS = 8
MLA_Q_RANK = 512
MLA_KV_RANK = 512
MLA_NOPE = 128
MLA_ROPE = 64
MLA_V = 128
DIFF_HEADS = 8
DIFF_QK = 64
DIFF_V = 2 * DIFF_QK
GQA_HEADS = 8
GQA_KV_HEADS = 2
GQA_HEAD_DIM = 128
WINDOW = 128
BAND_BLOCK = 128
SSD_HEADS = 16
SSD_HEAD_DIM = 64
SSD_D_INNER = SSD_HEADS * SSD_HEAD_DIM
SSD_GROUPS = 2
SSD_STATE = 128
SSD_CONV = 3
SSD_CHUNK = 128
SSD_CONV_DIM = SSD_D_INNER + 2 * SSD_GROUPS * SSD_STATE
MOE_GROUPS = 4
MOE_EXPERTS_PER_GROUP = 4
MOE_EXPERTS = MOE_GROUPS * MOE_EXPERTS_PER_GROUP
MOE_TOP_K = 2
MOE_HIDDEN = 512

EV_SPLITS = (MLA_Q_RANK, MLA_KV_RANK, MLA_ROPE, DIFF_HEADS * 2 * DIFF_QK, DIFF_HEADS * 2 * DIFF_QK, DIFF_HEADS * DIFF_V)
EV_IN = sum(EV_SPLITS)
EV_MIX = MLA_HEADS * MLA_V + DIFF_HEADS * DIFF_V
OD_SPLITS = (GQA_HEADS * GQA_HEAD_DIM, GQA_KV_HEADS * GQA_HEAD_DIM, GQA_KV_HEADS * GQA_HEAD_DIM, SSD_D_INNER, SSD_CONV_DIM, 2 * SSD_HEADS)
OD_IN = sum(OD_SPLITS)
OD_MIX = GQA_HEADS * GQA_HEAD_DIM + SSD_D_INNER

kernel_name = 'hybrid_diffusion_prefix_step'


def split_cols(x, sizes):
    return jnp.split(x, np.cumsum(sizes)[:-1].tolist(), axis=-1)


def rms_norm(x, g):
    xf = x.astype(jnp.float32)
    y = xf * lax.rsqrt(jnp.mean(xf * xf, axis=-1, keepdims=True) + RMS_EPS)
    return (y * g.astype(jnp.float32)).astype(x.dtype)


def layer_norm(x, g, b):
    xf = x.astype(jnp.float32)
    mu = jnp.mean(xf, axis=-1, keepdims=True)
    xc = xf - mu
    y = xc * lax.rsqrt(jnp.mean(xc * xc, axis=-1, keepdims=True) + LN_EPS)
    return (y * g.astype(jnp.float32) + b.astype(jnp.float32)).astype(x.dtype)


def adaln(cvec, w, b):
    m = jax.nn.silu(cvec) @ w + b
    return jnp.split(m[..., None, :], 6, axis=-1)


def modulate(x, shift, scale):
    return x * (1.0 + scale) + shift


def axial_rope(x):
    S, R = x.shape[1], x.shape[-1]
    rows = S // GRID_W
    row = jnp.repeat(jnp.arange(rows), GRID_W).astype(jnp.float32)
    col = (jnp.arange(S) % GRID_W).astype(jnp.float32)
    half = R // 2
    quarter = half // 2
    inv = ROPE_THETA ** (-jnp.arange(quarter, dtype=jnp.float32) * 2.0 / half)
    shape = (1, S) + (1,) * (x.ndim - 3) + (quarter,)

    def rot(xa, pos):
        ang = (pos[:, None] * inv[None, :]).reshape(shape)
        cos, sin = jnp.cos(ang), jnp.sin(ang)
        x1 = xa[..., :quarter].astype(jnp.float32)
        x2 = xa[..., quarter:].astype(jnp.float32)
        return jnp.concatenate([x1 * cos - x2 * sin, x2 * cos + x1 * sin], axis=-1)

    out = jnp.concatenate([rot(x[..., :half], row), rot(x[..., half:], col)], axis=-1)
    return out.astype(x.dtype)


def dense_attention(q, k, v, scale, sink=None):
    Bsz, Sq, H, dk = q.shape
    KVH, dv = k.shape[2], v.shape[-1]
    G = H // KVH
    nb = Sq // Q_BLOCK
    qb = q.reshape(Bsz, nb, Q_BLOCK, KVH, G, dk).transpose(1, 0, 2, 3, 4, 5)

    def one(qblk):
        s = jnp.einsum('bqkgd,bskd->bkgqs', qblk, k, preferred_element_type=jnp.float32) * scale
        if sink is not None:
            snk = jnp.broadcast_to(sink.astype(jnp.float32).reshape(1, KVH, G, 1, 1), s.shape[:-1] + (1,))
            s = jnp.concatenate([s, snk], axis=-1)
        p = jax.nn.softmax(s, axis=-1)
        if sink is not None:
            p = p[..., :-1]
        return jnp.einsum('bkgqs,bskd->bqkgd', p.astype(v.dtype), v)

    out = lax.map(one, qb)
    return out.transpose(1, 0, 2, 3, 4, 5).reshape(Bsz, Sq, H, dv)


def windowed_gqa(q, k, v, k_ctx, v_ctx, sink):
    Bsz, S, H, d = q.shape
    KVH = k.shape[2]
    G = H // KVH
    blk = BAND_BLOCK
    nb = S // blk
    scale = d ** -0.5
    qb = q.reshape(Bsz, nb, blk, KVH, G, d)
    idx = jnp.arange(nb)[:, None] * blk + jnp.arange(3 * blk)[None, :]
    pad = ((0, 0), (blk, blk), (0, 0), (0, 0))
    kb = jnp.pad(k, pad)[:, idx]
    vb = jnp.pad(v, pad)[:, idx]
    qpos = jnp.arange(nb)[:, None] * blk + jnp.arange(blk)[None, :]
    kpos = idx - blk
    valid = (jnp.abs(qpos[:, :, None] - kpos[:, None, :]) <= WINDOW) & (kpos[:, None, :] >= 0) & (kpos[:, None, :] < S)
    s_loc = jnp.einsum('bnqkgd,bnjkd->bnkgqj', qb, kb, preferred_element_type=jnp.float32) * scale
    s_loc = jnp.where(valid[None, :, None, None], s_loc, -jnp.inf)
    s_ctx = jnp.einsum('bnqkgd,bckd->bnkgqc', qb, k_ctx, preferred_element_type=jnp.float32) * scale
    s_snk = jnp.broadcast_to(sink.astype(jnp.float32).reshape(1, 1, KVH, G, 1, 1), s_loc.shape[:-1] + (1,))
    p = jax.nn.softmax(jnp.concatenate([s_loc, s_ctx, s_snk], axis=-1), axis=-1).astype(v.dtype)
    nloc, nctx = 3 * blk, k_ctx.shape[1]
    o = jnp.einsum('bnkgqj,bnjkd->bnqkgd', p[..., :nloc], vb) + jnp.einsum('bnkgqc,bckd->bnqkgd', p[..., nloc:nloc + nctx], v_ctx)
    return o.reshape(Bsz, S, H, d)


def centred_dwconv(x, w, b):
    kw = w.shape[0]
    y = lax.conv_general_dilated(x, w[:, None, :], window_strides=(1,), padding=[((kw - 1) // 2, kw // 2)],
                                 dimension_numbers=('NWC', 'WIO', 'NWC'), feature_group_count=x.shape[-1])
    return y + b


def ssd_scan(x, dt, a, bm, cm, h0):
    Bsz, S, H, P = x.shape
    N = bm.shape[-1]
    Q = SSD_CHUNK
    nc = S // Q
    xf = x.astype(jnp.float32).reshape(Bsz, nc, Q, H, P)
    dtc = dt.reshape(Bsz, nc, Q, H)
    bc = bm.astype(jnp.float32).reshape(Bsz, nc, Q, H, N)
    cc = cm.astype(jnp.float32).reshape(Bsz, nc, Q, H, N)
    cum = jnp.cumsum(dtc * a, axis=2)
    xdt = xf * dtc[..., None]
    tri = jnp.tril(jnp.ones((Q, Q), dtype=bool))[None, None, :, :, None]
    seg = cum[:, :, :, None, :] - cum[:, :, None, :, :]
    decay = jnp.exp(jnp.where(tri, seg, -jnp.inf))
    scores = jnp.einsum('bclhn,bcshn->bclsh', cc, bc) * decay
    y_diag = jnp.einsum('bclsh,bcshp->bclhp', scores, xdt)
    tail = jnp.exp(cum[:, :, -1:, :] - cum)
    chunk_states = jnp.einsum('bclhn,bclh,bclhp->bchpn', bc, tail, xdt)
    chunk_decay = jnp.exp(cum[:, :, -1, :])

    def step(hprev, inp):
        dec, st = inp
        return hprev * dec[:, :, None, None] + st, hprev

    h_final, h_in = lax.scan(step, h0.astype(jnp.float32),
                             (chunk_decay.transpose(1, 0, 2), chunk_states.transpose(1, 0, 2, 3, 4)))
    h_in = h_in.transpose(1, 0, 2, 3, 4)
    y_off = jnp.einsum('bclhn,bchpn,bclh->bclhp', cc, h_in, jnp.exp(cum))
    y = (y_diag + y_off).reshape(Bsz, S, H, P)
    return y.astype(x.dtype), h_final


def even_mixer(h, p, layer_idx, ctx):
    w_in, q_norm, kv_norm, wq_b, wkv_b, lam, subln, w_out = p
    Bsz, S, _ = h.shape
    q_lat, kv_lat, k_pe, dq, dk, dv = split_cols(h @ w_in, EV_SPLITS)
    q = (rms_norm(q_lat, q_norm) @ wq_b).reshape(Bsz, S, MLA_HEADS, MLA_NOPE + MLA_ROPE)
    c_kv = rms_norm(kv_lat, kv_norm)
    dq = dq.reshape(Bsz, S, DIFF_HEADS, 2, DIFF_QK)
    dk = dk.reshape(Bsz, S, DIFF_HEADS, 2, DIFF_QK)
    dv = dv.reshape(Bsz, S, DIFF_HEADS, DIFF_V)
    if ctx is None:
        cache = (c_kv, k_pe, dk.reshape(Bsz, S, DIFF_HEADS, 2 * DIFF_QK), dv)
        ckv_all, kpe_all, dk_all, dv_all = c_kv, k_pe, dk, dv
    else:
        ctx_ckv, ctx_kpe, ctx_dk, ctx_dv = ctx
        L = ctx_ckv.shape[1]
        q = jnp.concatenate([q[..., :MLA_NOPE], axial_rope(q[..., MLA_NOPE:])], axis=-1)
        k_pe = axial_rope(k_pe[:, :, None, :])[:, :, 0]
        dq, dk = axial_rope(dq), axial_rope(dk)
        ckv_all = jnp.concatenate([c_kv, ctx_ckv], axis=1)
        kpe_all = jnp.concatenate([k_pe, ctx_kpe], axis=1)
        dk_all = jnp.concatenate([dk, ctx_dk.reshape(Bsz, L, DIFF_HEADS, 2, DIFF_QK)], axis=1)
        dv_all = jnp.concatenate([dv, ctx_dv], axis=1)
        cache = None
    Sk = ckv_all.shape[1]
    kv = (ckv_all @ wkv_b).reshape(Bsz, Sk, MLA_HEADS, MLA_NOPE + MLA_V)
    k_mla = jnp.concatenate([kv[..., :MLA_NOPE], jnp.broadcast_to(kpe_all[:, :, None, :], (Bsz, Sk, MLA_HEADS, MLA_ROPE))], axis=-1)
    o_mla = dense_attention(q, k_mla, kv[..., MLA_NOPE:], (MLA_NOPE + MLA_ROPE) ** -0.5)
    lam_init = 0.8 - 0.6 * math.exp(-0.3 * layer_idx)
    lamf = lam.astype(jnp.float32)
    lam_full = jnp.exp(jnp.sum(lamf[0] * lamf[1])) - jnp.exp(jnp.sum(lamf[2] * lamf[3])) + lam_init
    a1 = dense_attention(dq[..., 0, :], dk_all[..., 0, :], dv_all, DIFF_QK ** -0.5)
    a2 = dense_attention(dq[..., 1, :], dk_all[..., 1, :], dv_all, DIFF_QK ** -0.5)
    o_diff = rms_norm(a1 - lam_full.astype(a1.dtype) * a2, subln) * (1.0 - lam_init)
    out = jnp.concatenate([o_mla.reshape(Bsz, S, -1), o_diff.reshape(Bsz, S, -1)], axis=-1) @ w_out
    return out, cache


def odd_mixer(h, p, ctx):
    w_in, sink, conv_w, conv_b, dt_bias, a_log, d_skip, norm_g, w_out = p
    Bsz, S, _ = h.shape
    q, k, v, z, xbc, dt = split_cols(h @ w_in, OD_SPLITS)
    q = q.reshape(Bsz, S, GQA_HEADS, GQA_HEAD_DIM)
    k = k.reshape(Bsz, S, GQA_KV_HEADS, GQA_HEAD_DIM)
    v = v.reshape(Bsz, S, GQA_KV_HEADS, GQA_HEAD_DIM)
    if ctx is None:
        o_att = dense_attention(q, k, v, GQA_HEAD_DIM ** -0.5, sink)
        h0_f = jnp.zeros((Bsz, SSD_HEADS, SSD_HEAD_DIM, SSD_STATE), jnp.float32)
        h0_b = h0_f
    else:
        k_ctx, v_ctx, h0_f, h0_b = ctx
        o_att = windowed_gqa(axial_rope(q), axial_rope(k), v, k_ctx, v_ctx, sink)
    xbc = jax.nn.silu(centred_dwconv(xbc, conv_w, conv_b))
    xs, bm, cm = split_cols(xbc, (SSD_D_INNER, SSD_GROUPS * SSD_STATE, SSD_GROUPS * SSD_STATE))
    xs = xs.reshape(Bsz, S, SSD_HEADS, SSD_HEAD_DIM)
    rep = SSD_HEADS // SSD_GROUPS
    bm = jnp.repeat(bm.reshape(Bsz, S, SSD_GROUPS, SSD_STATE), rep, axis=2)
    cm = jnp.repeat(cm.reshape(Bsz, S, SSD_GROUPS, SSD_STATE), rep, axis=2)
    dtp = jax.nn.softplus(dt.astype(jnp.float32).reshape(Bsz, S, 2, SSD_HEADS) + dt_bias.astype(jnp.float32))
    a = -jnp.exp(a_log.astype(jnp.float32))
    y_f, hf = ssd_scan(xs, dtp[:, :, 0], a[0], bm, cm, h0_f)
    y_b, hb = ssd_scan(jnp.flip(xs, 1), jnp.flip(dtp[:, :, 1], 1), a[1], jnp.flip(bm, 1), jnp.flip(cm, 1), h0_b)
    y = y_f + jnp.flip(y_b, 1) + xs * d_skip[:, None]
    y = y.reshape(Bsz, S, SSD_D_INNER) * jax.nn.silu(z)
    y = rms_norm(y.reshape(Bsz, S, SSD_GROUPS, -1), norm_g.reshape(SSD_GROUPS, -1)).reshape(Bsz, S, SSD_D_INNER)
    out = jnp.concatenate([o_att.reshape(Bsz, S, -1), y], axis=-1) @ w_out
    cache = (k, v, hf, hb) if ctx is None else None
    return out, cache


def hier_moe(h, w_rg, w_re, w_gate, w_up, w_down):
    Bsz, S, D = h.shape
    x = h.reshape(Bsz * S, D)
    pg = jax.nn.softmax(jnp.einsum('td,dg->tg', x, w_rg, preferred_element_type=jnp.float32), axis=-1)
    g_val, g_idx = lax.top_k(pg, 1)
    le = jnp.einsum('td,de->te', x, w_re, preferred_element_type=jnp.float32).reshape(-1, MOE_GROUPS, MOE_EXPERTS_PER_GROUP)
    le_sel = jnp.take_along_axis(le, g_idx[:, :, None], axis=1)[:, 0]
    e_val, e_idx = lax.top_k(jax.nn.softmax(le_sel, axis=-1), MOE_TOP_K)
    e_val = e_val / jnp.sum(e_val, axis=-1, keepdims=True)
    w_grp = jnp.sum(jax.nn.one_hot(e_idx, MOE_EXPERTS_PER_GROUP, dtype=jnp.float32) * e_val[..., None], axis=1)
    combine = (jax.nn.one_hot(g_idx[:, 0], MOE_GROUPS, dtype=jnp.float32)[:, :, None] * w_grp[:, None, :] * g_val[:, :, None]).reshape(-1, MOE_EXPERTS)
    hg = jnp.einsum('td,edf->tef', x, w_gate)
    hu = jnp.einsum('td,edf->tef', x, w_up)
    act = jax.nn.silu(hg) * hu * combine[..., None].astype(x.dtype)
    y = jnp.einsum('tef,efd->td', act, w_down)
    return y.reshape(Bsz, S, D)


def setup_inputs(seed: int = 0) -> dict:
    key = jax.random.key(seed)
    keys = jax.random.split(key, 64)
    counter = [0]

    def nk():
        k = keys[counter[0]]
        counter[0] += 1
        return k

    def nrm(shape, scale):
        return jax.random.normal(nk(), shape, jnp.float32) * scale

    D = D_MODEL
    L = PAST_LEN
    inp = {}
    inp['x_prompt'] = nrm((BATCH, SEQ, D), 1.0)
    inp['x_sample'] = nrm((DEC_BATCH, DEC_SEQ, D), 1.0)
    inp['cache_mla_ckv'] = nrm((DEC_BATCH, N_EVEN, L, MLA_KV_RANK), 1.0)
    inp['cache_mla_kpe'] = nrm((DEC_BATCH, N_EVEN, L, MLA_ROPE), 1.0)
    inp['cache_diff_k'] = nrm((DEC_BATCH, N_EVEN, L, DIFF_HEADS, 2 * DIFF_QK), 1.0)
    inp['cache_diff_v'] = nrm((DEC_BATCH, N_EVEN, L, DIFF_HEADS, DIFF_V), 1.0)
    inp['cache_gqa_k'] = nrm((DEC_BATCH, N_ODD, L, GQA_KV_HEADS, GQA_HEAD_DIM), 1.0)
    inp['cache_gqa_v'] = nrm((DEC_BATCH, N_ODD, L, GQA_KV_HEADS, GQA_HEAD_DIM), 1.0)
    inp['state_ssd_fwd'] = nrm((DEC_BATCH, N_ODD, SSD_HEADS, SSD_HEAD_DIM, SSD_STATE), 0.1)
    inp['state_ssd_bwd'] = nrm((DEC_BATCH, N_ODD, SSD_HEADS, SSD_HEAD_DIM, SSD_STATE), 0.1)
    inp['c'] = nrm((DEC_BATCH, D), 1.0)
    inp['c_ctx'] = nrm((D,), 1.0)
    inp['w_mod'] = nrm((DEPTH, D, 6 * D), 0.5 * D ** -0.5)
    inp['b_mod'] = nrm((DEPTH, 6 * D), 0.02)
    inp['ln1_g'] = 1.0 + nrm((DEPTH, D), 0.02)
    inp['ln1_b'] = nrm((DEPTH, D), 0.02)
    inp['ln2_g'] = 1.0 + nrm((DEPTH, D), 0.02)
    inp['ln2_b'] = nrm((DEPTH, D), 0.02)
    inp['ev_w_in'] = nrm((N_EVEN, D, EV_IN), D ** -0.5)
    inp['mla_q_norm'] = 1.0 + nrm((N_EVEN, MLA_Q_RANK), 0.02)
    inp['mla_kv_norm'] = 1.0 + nrm((N_EVEN, MLA_KV_RANK), 0.02)
    inp['mla_wq_b'] = nrm((N_EVEN, MLA_Q_RANK, MLA_HEADS * (MLA_NOPE + MLA_ROPE)), MLA_Q_RANK ** -0.5)
    inp['mla_wkv_b'] = nrm((N_EVEN, MLA_KV_RANK, MLA_HEADS * (MLA_NOPE + MLA_V)), MLA_KV_RANK ** -0.5)
    inp['diff_lambda'] = nrm((N_EVEN, 4, DIFF_QK), 0.1)
    inp['diff_subln'] = 1.0 + nrm((N_EVEN, DIFF_V), 0.02)
    inp['ev_w_out'] = nrm((N_EVEN, EV_MIX, D), DEEPNORM_BETA * EV_MIX ** -0.5)
    inp['od_w_in'] = nrm((N_ODD, D, OD_IN), D ** -0.5)
    inp['gqa_sink'] = nrm((N_ODD, GQA_HEADS), 0.5)
    inp['ssd_conv_w'] = nrm((N_ODD, SSD_CONV, SSD_CONV_DIM), SSD_CONV ** -0.5)
    inp['ssd_conv_b'] = nrm((N_ODD, SSD_CONV_DIM), 0.02)
    dt0 = jnp.exp(jax.random.uniform(nk(), (N_ODD, 2, SSD_HEADS), jnp.float32, math.log(1e-3), math.log(1e-1)))
    inp['ssd_dt_bias'] = dt0 + jnp.log(-jnp.expm1(-dt0))
    inp['ssd_a_log'] = jnp.log(jax.random.uniform(nk(), (N_ODD, 2, SSD_HEADS), jnp.float32, 1.0, 16.0))
    inp['ssd_d'] = 1.0 + nrm((N_ODD, SSD_HEADS), 0.02)
    inp['ssd_norm'] = 1.0 + nrm((N_ODD, SSD_D_INNER), 0.02)
    inp['od_w_out'] = nrm((N_ODD, OD_MIX, D), DEEPNORM_BETA * OD_MIX ** -0.5)
    inp['moe_router_group'] = nrm((DEPTH, D, MOE_GROUPS), D ** -0.5)
    inp['moe_router_expert'] = nrm((DEPTH, D, MOE_EXPERTS), D ** -0.5)
    inp['moe_w_gate'] = nrm((DEPTH, MOE_EXPERTS, D, MOE_HIDDEN), D ** -0.5)
    inp['moe_w_up'] = nrm((DEPTH, MOE_EXPERTS, D, MOE_HIDDEN), D ** -0.5)
    inp['moe_w_down'] = nrm((DEPTH, MOE_EXPERTS, MOE_HIDDEN, D), DEEPNORM_BETA * MOE_HIDDEN ** -0.5)
    return inp


def reference(x_prompt, x_sample, cache_mla_ckv, cache_mla_kpe, cache_diff_k, cache_diff_v, cache_gqa_k, cache_gqa_v,
              state_ssd_fwd, state_ssd_bwd, c, c_ctx, w_mod, b_mod, ln1_g, ln1_b, ln2_g, ln2_b,
              ev_w_in, mla_q_norm, mla_kv_norm, mla_wq_b, mla_wkv_b, diff_lambda, diff_subln, ev_w_out,
              od_w_in, gqa_sink, ssd_conv_w, ssd_conv_b, ssd_dt_bias, ssd_a_log, ssd_d, ssd_norm, od_w_out,
              moe_router_group, moe_router_expert, moe_w_gate, moe_w_up, moe_w_down):
    xp, xs = x_prompt, x_sample
    ev_caches, od_caches = [], []
    for l in range(DEPTH):
        i = l // 2
        mp = adaln(c_ctx, w_mod[l], b_mod[l])
        ms = adaln(c, w_mod[l], b_mod[l])
        hp = modulate(xp, mp[0], mp[1])
        hs = modulate(xs, ms[0], ms[1])
        if l % 2 == 0:
            p = (ev_w_in[i], mla_q_norm[i], mla_kv_norm[i], mla_wq_b[i], mla_wkv_b[i], diff_lambda[i], diff_subln[i], ev_w_out[i])
            op, cache = even_mixer(hp, p, l, None)
            os_, _ = even_mixer(hs, p, l, (cache_mla_ckv[:, i], cache_mla_kpe[:, i], cache_diff_k[:, i], cache_diff_v[:, i]))
            ev_caches.append(cache)
        else:
            p = (od_w_in[i], gqa_sink[i], ssd_conv_w[i], ssd_conv_b[i], ssd_dt_bias[i], ssd_a_log[i], ssd_d[i], ssd_norm[i], od_w_out[i])
            op, cache = odd_mixer(hp, p, None)
            os_, _ = odd_mixer(hs, p, (cache_gqa_k[:, i], cache_gqa_v[:, i], state_ssd_fwd[:, i], state_ssd_bwd[:, i]))
            od_caches.append(cache)
        xp = layer_norm(DEEPNORM_ALPHA * xp + mp[2] * op, ln1_g[l], ln1_b[l])
        xs = layer_norm(DEEPNORM_ALPHA * xs + ms[2] * os_, ln1_g[l], ln1_b[l])
        moe_p = (moe_router_group[l], moe_router_expert[l], moe_w_gate[l], moe_w_up[l], moe_w_down[l])
        xp = layer_norm(DEEPNORM_ALPHA * xp + mp[5] * hier_moe(modulate(xp, mp[3], mp[4]), *moe_p), ln2_g[l], ln2_b[l])
        xs = layer_norm(DEEPNORM_ALPHA * xs + ms[5] * hier_moe(modulate(xs, ms[3], ms[4]), *moe_p), ln2_g[l], ln2_b[l])
    new_mla_ckv = jnp.stack([t[0] for t in ev_caches], axis=1)
    new_mla_kpe = jnp.stack([t[1] for t in ev_caches], axis=1)
    new_diff_k = jnp.stack([t[2] for t in ev_caches], axis=1)
    new_diff_v = jnp.stack([t[3] for t in ev_caches], axis=1)
    new_gqa_k = jnp.stack([t[0] for t in od_caches], axis=1)
    new_gqa_v = jnp.stack([t[1] for t in od_caches], axis=1)
    new_ssd_fwd = jnp.stack([t[2] for t in od_caches], axis=1)
    new_ssd_bwd = jnp.stack([t[3] for t in od_caches], axis=1)
    return (xp, xs, new_mla_ckv, new_mla_kpe, new_diff_k, new_diff_v, new_gqa_k, new_gqa_v, new_ssd_fwd, new_ssd_bwd)
```

```python
import numpy as np
from contextlib import ExitStack
import concourse.bass as bass
import concourse.mybir as mybir
from concourse.bass_utils import run_bass_kernel_spmd

F32 = mybir.dt.float32
BF16 = mybir.dt.bfloat16
AF = mybir.ActivationFunctionType
ALU = mybir.AluOpType
AX = mybir.AxisListType

D = 2048
KC = 16
DEPTH = 4
ALPHA = (2.0 * DEPTH) ** 0.25
LN_EPS = 1e-5
RMS_EPS = 1e-6
NE = 16


class Tl:
    __slots__ = ("ap", "w", "r", "name")

    def __init__(self, ap, name=""):
        self.ap = ap
        self.w = None
        self.r = []
        self.name = name

    def __getitem__(self, k):
        return self.ap[k]


class DSem:
    __slots__ = ("h", "cnt", "idx")

    def __init__(self, h, idx):
        self.h = h
        self.cnt = 0
        self.idx = idx


class Prog:
    ENG = ("pe", "act", "dve", "pool", "sp")

    def __init__(self, nc):
        self.nc = nc
        self.es = ExitStack()
        self.E = {"pe": nc.tensor, "act": nc.scalar, "dve": nc.vector, "pool": nc.gpsimd, "sp": nc.sync}
        self.sem = {e: self.es.enter_context(nc.semaphore("sem_" + e)) for e in self.ENG}
        self.cnt = {e: 0 for e in self.ENG}
        self.known = {e: {} for e in self.ENG}
        self.dsems = []
        self.semh = {("e", e): self.sem[e] for e in self.ENG}
        self.ninstr = 0
        self._uid = 0
        self.log = None
        self.lastwaits = []

    def uid(self, s):
        self._uid += 1
        return "%s_%d" % (s, self._uid)

    def dsem(self):
        idx = len(self.dsems)
        h = self.es.enter_context(self.nc.semaphore("dsem%d" % idx))
        d = DSem(h, idx)
        self.dsems.append(d)
        self.semh[("d", idx)] = h
        return d

    def sb(self, es, shape, dt, name="t"):
        return es.enter_context(self.nc.sbuf_tensor(self.uid(name), list(shape), dt))

    def tile(self, es, shape, dt, name="t"):
        t = self.sb(es, shape, dt, name)
        return Tl(t[:] if False else t, name)

    def _deps(self, eng, reads, writes):
        deps = {}

        def need(ev):
            if ev is None:
                return
            k, v = ev
            if deps.get(k, 0) < v:
                deps[k] = v

        for t in reads:
            need(t.w)
        for t in writes:
            need(t.w)
            for ev in t.r:
                need(ev)
        E = self.E[eng]
        kn = self.known[eng]
        self.lastwaits = []
        for k, v in deps.items():
            if k == ("e", "pe") and eng == "pe":
                continue
            if kn.get(k, 0) >= v:
                continue
            kn[k] = v
            E.wait_ge(self.semh[k], v)
            self.lastwaits.append((k, v))
            self.ninstr += 1

    def _mark(self, ev, reads, writes):
        for t in reads:
            t.r.append(ev)
        for t in writes:
            t.w = ev
            t.r = []

    def op(self, eng, fn, r=(), w=()):
        self._deps(eng, r, w)
        ins = fn(self.E[eng])
        self.cnt[eng] += 1
        ins.then_inc(self.sem[eng], 1)
        if self.log is not None:
            self.log.append((eng, self.cnt[eng], fn.__code__.co_firstlineno, list(self.lastwaits)))
        self.ninstr += 1
        self._mark((("e", eng), self.cnt[eng]), r, w)

    def pe(self, fn, r=(), w=()):
        self.op("pe", fn, r, w)

    def act(self, fn, r=(), w=()):
        self.op("act", fn, r, w)

    def dve(self, fn, r=(), w=()):
        self.op("dve", fn, r, w)

    def pool(self, fn, r=(), w=()):
        self.op("pool", fn, r, w)

    def dma(self, q, ds, out, in_, r=(), w=()):
        self._deps(q, r, w)
        ins = self.E[q].dma_start(out=out, in_=in_)
        ds.cnt += 1
        ins.then_inc(ds.h, 16)
        if self.log is not None:
            self.log.append((q + "-dma", ("d", ds.idx, 16 * ds.cnt), 0, list(self.lastwaits)))
        self.ninstr += 1
        self._mark((("d", ds.idx), 16 * ds.cnt), r, w)

    def dma_group(self, q, ds, items):
        tiles = []
        for out, in_, w in items:
            self.dma(q, ds, out, in_, w=w)
            tiles += list(w)
        ev = (("d", ds.idx), 16 * ds.cnt)
        for t in tiles:
            t.w = ev

    def barrier(self):
        for e in self.ENG:
            kn = self.known[e]
            for o in self.ENG:
                v = self.cnt[o]
                if o == "pe" and e == "pe":
                    continue
                if v > 0 and kn.get(("e", o), 0) < v:
                    kn[("e", o)] = v
                    self.E[e].wait_ge(self.sem[o], v)
            for d in self.dsems:
                v = 16 * d.cnt
                if v > 0 and kn.get(("d", d.idx), 0) < v:
                    kn[("d", d.idx)] = v
                    self.E[e].wait_ge(d.h, v)

    def finish(self):
        e = "sp"
        for o in self.ENG:
            if o != e and self.cnt[o] > 0:
                self.E[e].wait_ge(self.sem[o], self.cnt[o])
        for d in self.dsems:
            if d.cnt > 0:
                self.E[e].wait_ge(d.h, 16 * d.cnt)


OPT = {}


class Grp:
    def __init__(self, name, T, nseq, Sq, L, vidx):
        self.name, self.T, self.nseq, self.Sq, self.L, self.vidx = name, T, nseq, Sq, L, vidx
        self.smp = L > 0
        self.nk = Sq + L
        self.ntt = T // 128
        self.ntb = T // 512


GP = Grp("P", 512, 2, 256, 0, 0)
GS = Grp("S", 1024, 1, 1024, 512, 1)


class K:
    pass


def build(depth=DEPTH, groups=("P", "S"), dbg=False):
    nc = bass.Bass("TRN2", target_bir_lowering=False)
    P = Prog(nc)
    dr = {}

    def din(name, shape):
        dr[name] = nc.dram_tensor(name, list(shape), F32, kind="ExternalInput").ap()

    def dout(name, shape):
        dr[name] = nc.dram_tensor(name, list(shape), F32, kind="ExternalOutput").ap()

    din("xpT", [D, 512]); din("xsT", [D, 1024]); din("cT", [128, 16, 2])
    din("w_mod", [depth, D, 6 * D]); din("b_modT", [128, 4, 96]); din("lnp", [128, 4, 4, 16])
    din("ev_w_in", [2, D, 4160]); din("mla_wq_b", [2, 512, 1536]); din("mla_wkv_b", [2, 512, 2048])
    din("ev_w_out", [2, D, D]); din("bvec_ev", [128, 2, 1408])
    din("od_w_in", [2, D, 4128]); din("od_w_out", [2, D, D]); din("bvec_od", [128, 2, 1112])
    din("convp", [128, 2, 12, 4])
    din("moe_rt", [4, D, 20]); din("moe_w_gate", [depth, 16, D, 512]); din("moe_w_up", [depth, 16, D, 512])
    din("moe_w_down", [depth, 16, 512, D]); din("selE", [16, 16, 128])
    din("ckv_ctxT", [2, 512, 512]); din("kpe_ctxT2", [2, 128, 512]); din("dk_ctxT", [2, 8, 128, 512])
    din("dv_ctx", [2, 512, 1024]); din("gk_ctxT", [2, 2, 128, 512]); din("gv_ctx", [2, 512, 256])
    din("ssd_h0", [2, 2, 16, 64, 128])
    din("rope64", [128, 8, 2, 64]); din("rope128", [128, 8, 2, 128]); din("tri", [128, 4, 128])
    dout("ypT", [D, 512]); dout("ysT", [D, 1024])
    dout("nckv", [2, 2, 256, 512]); dout("nkpe", [2, 2, 256, 64]); dout("ndk", [2, 2, 256, 1024])
    dout("ndv", [2, 2, 256, 1024]); dout("ngk", [2, 2, 256, 256]); dout("ngv", [2, 2, 256, 256])
    dout("nssd", [2, 2, 2, 16, 128, 64])

    with P.es as ges:
        k = K()
        k.nc, k.P, k.dr, k.depth = nc, P, dr, depth
        k.psf = [Tl(ges.enter_context(nc.psum_tensor("psf%d" % i, [128, 512], F32)), "psf") for i in range(6)]
        k.psb = [Tl(ges.enter_context(nc.psum_tensor("psb%d" % i, [128, 1024], BF16)), "psb") for i in range(2)]
        k.psi = [0, 0]

        k.psrot = list(k.psf)

        def ps():
            k.psi[0] += 1
            return k.psrot[k.psi[0] % len(k.psrot)]

        def psb():
            k.psi[1] += 1
            return k.psb[k.psi[1] % 2]
        k.ps, k.psbf = ps, psb
        k.identf = P.tile(ges, [128, 128], F32, "identf")
        k.identb = P.tile(ges, [128, 128], BF16, "identb")
        k.onesf = P.tile(ges, [128, 128], F32, "onesf")
        P.pool(lambda e: e.memset(k.onesf[:], 1.0), w=[k.onesf])
        P.pool(lambda e: e.memset(k.identf[:], 1.0), w=[k.identf])
        P.pool(lambda e: e.affine_select(out=k.identf[:], in_=k.identf[:], pattern=[[-1, 128]], compare_op=ALU.is_equal,
                                         fill=0.0, base=0, channel_multiplier=1), r=[k.identf], w=[k.identf])
        P.dve(lambda e: e.tensor_copy(out=k.identb[:], in_=k.identf[:]), r=[k.identf], w=[k.identb])
        k.modv = P.tile(ges, [128, 4, 112, 2], F32, "modv")
        k.lnp = P.tile(ges, [128, 4, 4, 16], F32, "lnp")
        k.cds = P.dsem()
        P.dma("sp", k.cds, k.lnp[:], dr["lnp"][:, :, :, :], w=[k.lnp])
        k.wds = [P.dsem() for _ in range(4)]
        k.wi = [0]
        k.iods = [P.dsem() for _ in range(4)]
        k.mds = [P.dsem() for _ in range(16)]
        k.hds = [P.dsem() for _ in range(6)]

        if OPT.get("adaln", 1):
            adaln_phase(k)
        for gname in groups:
            g = GP if gname == "P" else GS
            run_group(k, g)
        P.finish()
    return nc, P


def dbg(k, name, tile, ap=None):
    if not OPT.get("dbg"):
        return
    ap = tile.ap[:] if ap is None else ap
    shp = list(ap.shape)
    d = k.nc.dram_tensor("dbg_" + name, shp, F32, kind="ExternalOutput").ap()
    if not hasattr(k, "dbgds"):
        k.dbgds = k.P.dsem()
    idx = tuple(slice(None) for _ in shp)
    k.P.dma("pool", k.dbgds, d[idx], ap, r=[tile])
    k.P.barrier()


def alloc_wslots(k, es, n, size=8192):
    k.wslots = [k.P.tile(es, [128, size], BF16, "wslot") for _ in range(n)]


def wslot(k):
    k.wi[0] += 1
    i = k.wi[0] % len(k.wslots)
    return k.wslots[i], k.wds[i]


def adaln_phase(k):
    P, dr = k.P, k.dr
    with ExitStack() as es:
        cf = P.tile(es, [128, 16, 2], F32, "cf")
        sc = P.tile(es, [128, 16, 2], BF16, "sc")
        bm = P.tile(es, [128, 4, 96], F32, "bm")
        alloc_wslots(k, es, 4)
        P.dma_group("sp", k.cds, [(cf[:], dr["cT"][:, :, :], [cf]), (bm[:], dr["b_modT"][:, :, :], [bm])])
        P.act(lambda e: e.activation(out=sc[:], in_=cf[:], func=AF.Silu), r=[cf], w=[sc])
        for l in range(k.depth):
            pb = k.ps()
            for cb in range(24):
                wt, ds = wslot(k)
                wv = wt.ap[:, :].rearrange("p (k c) -> p k c", k=16)
                src = dr["w_mod"][l, :, cb * 512:(cb + 1) * 512].rearrange("(k p) c -> p k c", p=128)
                P.dma("pool", ds, wv, src, w=[wt])
                for sub in range(4):
                    j = cb * 4 + sub
                    for kk in range(16):
                        P.pe(lambda e, kk=kk, j=j, sub=sub, wv=wv: e.matmul(
                            pb[:, 2 * j:2 * j + 2], lhsT=wv[:, kk, sub * 128:(sub + 1) * 128], rhs=sc[:, kk, :],
                            start=(kk == 0), stop=(kk == 15)), r=[wt, sc], w=[pb])
            mv = k.modv.ap[:, l, 0:96, :]
            P.dve(lambda e, l=l, pb=pb, mv=mv: e.tensor_tensor(
                out=mv, in0=pb[:, 0:192].rearrange("p (j v) -> p j v", v=2),
                in1=bm[:, l, :].unsqueeze(2).to_broadcast([128, 96, 2]), op=ALU.add), r=[pb, bm], w=[k.modv])
            for c0 in (16, 64):
                mv2 = k.modv.ap[:, l, c0:c0 + 16, :]
                P.dve(lambda e, mv2=mv2: e.tensor_scalar_add(out=mv2, in0=mv2, scalar1=1.0), r=[k.modv], w=[k.modv])
            mv3 = k.modv.ap[:, l, 96:112, :]
            mv1 = k.modv.ap[:, l, 16:32, :]
            P.dve(lambda e, mv3=mv3, mv1=mv1: e.tensor_single_scalar(out=mv3, in_=mv1, scalar=1.0 / ALPHA, op=ALU.mult), r=[k.modv], w=[k.modv])
        P.barrier()


def mod(k, l, comp, kk, v):
    return k.modv.ap[:, l, comp * 16 + kk, v:v + 1]


def run_group(k, g):
    P, dr = k.P, k.dr
    T = g.T
    with ExitStack() as es:
        xs_ = P.sb(es, [128, 16, T], F32, "x" + g.name)
        k.x = xs_
        k.xt = [[Tl(xs_[:, kk, tb * 512:(tb + 1) * 512], "x") for tb in range(g.ntb)] for kk in range(16)]
        src = dr["xpT" if g.name == "P" else "xsT"].rearrange("(k p) t -> p k t", p=128)
        allx = [t for row in k.xt for t in row]
        P.dma_group("sp", k.iods[0], [(xs_[:, kk, :], src[:, kk, :], k.xt[kk]) for kk in range(16)])
        for l in range(k.depth):
            if OPT.get("mixer", 1):
                if l % 2 == 0:
                    even_mixer(k, g, l)
                else:
                    odd_mixer(k, g, l)
            if OPT.get("ln", 1):
                layer_norm(k, g, l, 0)
            if OPT.get("moe", 1):
                moe(k, g, l)
            if OPT.get("ln", 1):
                layer_norm(k, g, l, 1)
        dst = dr["ypT" if g.name == "P" else "ysT"].rearrange("(k p) t -> p k t", p=128)
        for kk in range(16):
            P.dma("sp", k.iods[1], dst[:, kk, :], xs_[:, kk, :], r=k.xt[kk])
        P.barrier()


def layer_norm(k, g, l, which):
    P = k.P
    with ExitStack() as es:
        sq = [P.tile(es, [128, 512], F32, "lnsq") for _ in range(2)]
        mean = P.tile(es, [128, 512], F32, "lnmean")
        rstd = P.tile(es, [128, 512], F32, "lnrstd")
        tmp = [P.tile(es, [128, 512], F32, "lntmp") for _ in range(2)]
        for tb in range(g.ntb):
            pa, pb_ = k.ps(), k.ps()
            for kk in range(16):
                xt = k.xt[kk][tb]
                s = sq[kk % 2]
                P.act(lambda e, s=s, xt=xt: e.activation(out=s[:], in_=xt.ap, func=AF.Square), r=[xt], w=[s])
                P.pe(lambda e, s=s, kk=kk, pa=pa: e.matmul(pa[:], lhsT=k.onesf[:], rhs=s[:], start=(kk == 0), stop=(kk == 15)),
                     r=[s, k.onesf], w=[pa])
                P.pe(lambda e, xt=xt, kk=kk, pb_=pb_: e.matmul(pb_[:], lhsT=k.onesf[:], rhs=xt.ap, start=(kk == 0), stop=(kk == 15)),
                     r=[xt, k.onesf], w=[pb_])
            P.dve(lambda e, pb_=pb_: e.tensor_single_scalar(out=mean[:], in_=pb_[:], scalar=1.0 / D, op=ALU.mult), r=[pb_], w=[mean])
            P.dve(lambda e: e.tensor_tensor(out=rstd[:], in0=mean[:], in1=mean[:], op=ALU.mult), r=[mean], w=[rstd])
            P.dve(lambda e, pa=pa: e.scalar_tensor_tensor(out=rstd[:], in0=pa[:], scalar=1.0 / D, in1=rstd[:], op0=ALU.mult, op1=ALU.subtract),
                  r=[pa, rstd], w=[rstd])
            P.act(lambda e: e.activation(out=rstd[:], in_=rstd[:], func=AF.Sqrt, bias=LN_EPS, scale=1.0), r=[rstd], w=[rstd])
            P.dve(lambda e: e.reciprocal(out=rstd[:], in_=rstd[:]), r=[rstd], w=[rstd])
            for kk in range(16):
                xt = k.xt[kk][tb]
                t = tmp[kk % 2]
                P.dve(lambda e, t=t, xt=xt: e.tensor_tensor(out=t[:], in0=xt.ap, in1=mean[:], op=ALU.subtract), r=[xt, mean], w=[t])
                P.pool(lambda e, t=t: e.tensor_tensor(out=t[:], in0=t[:], in1=rstd[:], op=ALU.mult), r=[t, rstd], w=[t])
                P.act(lambda e, t=t, xt=xt, kk=kk: e.activation(out=xt.ap, in_=t[:], func=AF.Identity,
                                                               bias=k.lnp[:, l, 2 * which + 1, kk:kk + 1],
                                                               scale=k.lnp[:, l, 2 * which, kk:kk + 1]), r=[t, k.lnp], w=[xt])
        P.barrier()


def moe(k, g, l):
    P, dr = k.P, k.dr
    T, v = g.T, g.vidx
    with ExitStack() as es:
        h2 = P.sb(es, [128, 16, T], BF16, "h2T")
        h2t = [[Tl(h2[:, kk, tb * 512:(tb + 1) * 512], "h2") for tb in range(g.ntb)] for kk in range(16)]
        combT = [P.tile(es, [16, 512], F32, "combT") for _ in range(g.ntb)]
        rtw = P.tile(es, [128, 16, 20], F32, "rtw")
        k.selE = P.tile(es, [16, 16, 128], F32, "selE")
        P.dma("sp", k.mds[0], k.selE[:], dr["selE"][:, :, :], w=[k.selE])
        P.dma("sp", k.cds, rtw[:], dr["moe_rt"][l].rearrange("(k p) c -> p k c", p=128), w=[rtw])
        h2f = [P.tile(es, [128, 512], F32, "h2f") for _ in range(2)]
        at = P.sb(es, [128, 4, T], BF16, "actT")
        att = [[Tl(at[:, fc, tb * 512:(tb + 1) * 512], "act") for tb in range(g.ntb)] for fc in range(4)]
        lgT = P.tile(es, [20, 512], F32, "lgT")
        lg = P.tile(es, [128, 4, 20], F32, "lg")
        sm = P.tile(es, [128, 8, 4], F32, "rsm")
        o3 = P.tile(es, [128, 6, 4, 4], F32, "r3")
        t4 = P.tile(es, [128, 4, 4, 4], F32, "r4")
        comb = P.tile(es, [128, 4, 16], F32, "comb")
        sgs = [P.tile(es, [128, 512], F32, "sgs") for _ in range(2)]
        tus = [P.tile(es, [128, 512], F32, "tus") for _ in range(2)]
        cbs = [P.tile(es, [128, 512], F32, "cbs") for _ in range(2)]
        alloc_wslots(k, es, 4)

        def bc3(ap):
            return ap.unsqueeze(2).to_broadcast([128, 4, 4])
        for tb in range(g.ntb):
            pr = k.ps()
            for kk in range(16):
                xt = k.xt[kk][tb]
                hf = h2f[kk % 2]
                P.act(lambda e, hf=hf, xt=xt, kk=kk: e.activation(out=hf[:], in_=xt.ap, func=AF.Identity,
                                                                 bias=mod(k, l, 3, kk, v), scale=mod(k, l, 4, kk, v)),
                      r=[xt, k.modv], w=[hf])
                P.pool(lambda e, hf=hf, kk=kk, tb=tb: e.tensor_copy(out=h2t[kk][tb].ap, in_=hf[:]), r=[hf], w=[h2t[kk][tb]])
                P.pe(lambda e, hf=hf, kk=kk, pr=pr: e.matmul(pr[0:20, :], lhsT=rtw[:, kk, :], rhs=hf[:], start=(kk == 0), stop=(kk == 15)),
                     r=[rtw, hf], w=[pr])
                P.pool(lambda e, xt=xt: e.tensor_single_scalar(out=xt.ap, in_=xt.ap, scalar=ALPHA, op=ALU.mult), r=[xt], w=[xt])
            P.act(lambda e, pr=pr: e.copy(out=lgT[:], in_=pr[0:20, :]), r=[pr], w=[lgT])
            pt = k.ps()
            for j in range(4):
                P.pe(lambda e, j=j, pt=pt: e.transpose(pt[:, j * 20:(j + 1) * 20], lgT[0:20, j * 128:(j + 1) * 128], k.identf[0:20, 0:20]),
                     r=[lgT, k.identf], w=[pt])
            P.act(lambda e, pt=pt: e.copy(out=lg[:], in_=pt[:, 0:80].rearrange("p (j c) -> p j c", c=20)), r=[pt], w=[lg])
            lgG = lg.ap[:, :, 0:4]
            lgE = lg.ap[:, :, 4:20].rearrange("p j (g e) -> p j g e", e=4)
            mg, sg_, gval, me, m2, den, gw = [sm.ap[:, i, :] for i in range(7)]
            ohg, eg, lesel, ee, m1m, ee2 = [o3.ap[:, i, :, :] for i in range(6)]
            R = [lg, sm, o3, t4]
            W = [sm, o3, t4]
            dv = lambda fn: P.dve(fn, r=R, w=W)
            dv(lambda e: e.tensor_reduce(out=mg, in_=lgG, axis=AX.X, op=ALU.max))
            dv(lambda e: e.tensor_tensor(out=ohg, in0=lgG, in1=bc3(mg), op=ALU.is_ge))
            dv(lambda e: e.tensor_tensor(out=eg, in0=lgG, in1=bc3(mg), op=ALU.subtract))
            P.act(lambda e: e.activation(out=eg, in_=eg, func=AF.Exp), r=R, w=W)
            dv(lambda e: e.tensor_reduce(out=sg_, in_=eg, axis=AX.X, op=ALU.add))
            dv(lambda e: e.reciprocal(out=gval, in_=sg_))
            dv(lambda e: e.tensor_tensor(out=t4[:], in0=lgE, in1=ohg.unsqueeze(3).to_broadcast([128, 4, 4, 4]), op=ALU.mult))
            dv(lambda e: e.tensor_reduce(out=lesel, in_=t4.ap[:, :, :, :].rearrange("p j g e -> p j e g"), axis=AX.X, op=ALU.add))
            dv(lambda e: e.tensor_reduce(out=me, in_=lesel, axis=AX.X, op=ALU.max))
            dv(lambda e: e.tensor_tensor(out=m1m, in0=lesel, in1=bc3(me), op=ALU.is_ge))
            dv(lambda e: e.tensor_tensor(out=ee, in0=lesel, in1=bc3(me), op=ALU.subtract))
            P.act(lambda e: e.activation(out=ee, in_=ee, func=AF.Exp), r=R, w=W)
            dv(lambda e: e.scalar_tensor_tensor(out=ee2, in0=m1m, scalar=-2.0, in1=ee, op0=ALU.mult, op1=ALU.add))
            dv(lambda e: e.tensor_reduce(out=m2, in_=ee2, axis=AX.X, op=ALU.max))
            dv(lambda e: e.tensor_tensor(out=ee2, in0=ee2, in1=bc3(m2), op=ALU.is_ge))
            dv(lambda e: e.tensor_scalar_add(out=den, in0=m2, scalar1=1.0))
            dv(lambda e: e.reciprocal(out=den, in_=den))
            dv(lambda e: e.tensor_tensor(out=gw, in0=gval, in1=den, op=ALU.mult))
            dv(lambda e: e.tensor_tensor(out=ee2, in0=ee2, in1=bc3(m2), op=ALU.mult))
            dv(lambda e: e.tensor_tensor(out=ee2, in0=ee2, in1=m1m, op=ALU.add))
            dv(lambda e: e.tensor_tensor(out=ee2, in0=ee2, in1=bc3(gw), op=ALU.mult))
            P.dve(lambda e: e.tensor_tensor(out=comb.ap[:, :, :].rearrange("p j (g e) -> p j g e", e=4),
                                            in0=ohg.unsqueeze(3).to_broadcast([128, 4, 4, 4]),
                                            in1=ee2.unsqueeze(2).to_broadcast([128, 4, 4, 4]), op=ALU.mult), r=R, w=[comb])
            pc = k.ps()
            for j in range(4):
                P.pe(lambda e, j=j, pc=pc: e.transpose(pc[0:16, j * 128:(j + 1) * 128], comb[:, j, :], k.identf[:]),
                     r=[comb, k.identf], w=[pc])
            P.act(lambda e, pc=pc, tb=tb: e.copy(out=combT[tb][:], in_=pc[0:16, :]), r=[pc], w=[combT[tb]])
        n = 0
        for ex in range(OPT.get("nexp", NE)):
            wg, dg = wslot(k)
            wgv = wg.ap[:, :].rearrange("p (k c) -> p k c", k=16)
            P.dma("pool", dg, wgv, dr["moe_w_gate"][l, ex].rearrange("(k p) c -> p k c", p=128), w=[wg])
            wu, du = wslot(k)
            wuv = wu.ap[:, :].rearrange("p (k c) -> p k c", k=16)
            P.dma("pool", du, wuv, dr["moe_w_up"][l, ex].rearrange("(k p) c -> p k c", p=128), w=[wu])
            wd, dd = wslot(k)
            wdv = wd.ap[:, :].rearrange("p (k c) -> p k c", k=4)
            P.dma("pool", dd, wdv, dr["moe_w_down"][l, ex].rearrange("(k p) c -> p k c", p=128), w=[wd])
            for tb in range(g.ntb):
                n += 1
                pcb = k.ps()
                cb = cbs[n % 2]
                P.pe(lambda e, pcb=pcb, ex=ex, tb=tb: e.matmul(pcb[:], lhsT=k.selE[:, ex, :], rhs=combT[tb][:], start=True, stop=True),
                     r=[k.selE, combT[tb]], w=[pcb])
                P.act(lambda e, pcb=pcb, cb=cb: e.copy(out=cb[:], in_=pcb[:]), r=[pcb], w=[cb])
                for fc in range(4):
                    pg, pu = k.ps(), k.ps()
                    for kk in range(16):
                        P.pe(lambda e, pg=pg, kk=kk, fc=fc, tb=tb, wgv=wgv: e.matmul(
                            pg[:], lhsT=wgv[:, kk, fc * 128:(fc + 1) * 128], rhs=h2t[kk][tb].ap, start=(kk == 0), stop=(kk == 15)),
                            r=[wg, h2t[kk][tb]], w=[pg])
                    for kk in range(16):
                        P.pe(lambda e, pu=pu, kk=kk, fc=fc, tb=tb, wuv=wuv: e.matmul(
                            pu[:], lhsT=wuv[:, kk, fc * 128:(fc + 1) * 128], rhs=h2t[kk][tb].ap, start=(kk == 0), stop=(kk == 15)),
                            r=[wu, h2t[kk][tb]], w=[pu])
                    s_ = sgs[fc % 2]
                    t_ = tus[fc % 2]
                    P.act(lambda e, s_=s_, pg=pg: e.activation(out=s_[:], in_=pg[:], func=AF.Silu), r=[pg], w=[s_])
                    P.dve(lambda e, t_=t_, s_=s_, pu=pu: e.tensor_tensor(out=t_[:], in0=pu[:], in1=s_[:], op=ALU.mult), r=[pu, s_], w=[t_])
                    P.pool(lambda e, t_=t_, cb=cb, fc=fc, tb=tb: e.tensor_tensor(out=att[fc][tb].ap, in0=t_[:], in1=cb[:], op=ALU.mult),
                           r=[t_, cb], w=[att[fc][tb]])
                for oc in range(16):
                    pd = k.ps()
                    for fc in range(4):
                        P.pe(lambda e, pd=pd, fc=fc, oc=oc, tb=tb, wdv=wdv: e.matmul(
                            pd[:], lhsT=wdv[:, fc, oc * 128:(oc + 1) * 128], rhs=att[fc][tb].ap, start=(fc == 0), stop=(fc == 3)),
                            r=[wd, att[fc][tb]], w=[pd])
                    xt = k.xt[oc][tb]
                    P.dve(lambda e, pd=pd, xt=xt, oc=oc: e.scalar_tensor_tensor(
                        out=xt.ap, in0=pd[:], scalar=mod(k, l, 5, oc, v), in1=xt.ap, op0=ALU.mult, op1=ALU.add),
                        r=[pd, xt, k.modv], w=[xt])
        P.barrier()


class Scr:
    def __init__(self, P, es, nkmax):
        self.sm = P.tile(es, [128, 16], F32, "smx")
        self.Pe = P.tile(es, [128, nkmax], BF16, "Pe")


def softmax_core(k, sc, banks, widths, scale, sink=None, sink_tile=None):
    P = k.P
    sm = sc.sm
    nb_ = len(banks)
    for i, (b, w) in enumerate(zip(banks, widths)):
        P.dve(lambda e, i=i, b=b, w=w: e.tensor_reduce(out=sm[:, i:i + 1], in_=b[:, 0:w], axis=AX.X, op=ALU.max), r=[b], w=[sm])
    if nb_ > 1:
        P.dve(lambda e: e.tensor_reduce(out=sm[:, 4:5], in_=sm[:, 0:nb_], axis=AX.X, op=ALU.max), r=[sm], w=[sm])
    else:
        P.dve(lambda e: e.tensor_copy(out=sm[:, 4:5], in_=sm[:, 0:1]), r=[sm], w=[sm])
    if sink is not None:
        P.dve(lambda e: e.tensor_single_scalar(out=sm[:, 4:5], in_=sm[:, 4:5], scalar=scale, op=ALU.mult), r=[sm], w=[sm])
        P.dve(lambda e: e.tensor_tensor(out=sm[:, 4:5], in0=sm[:, 4:5], in1=sink, op=ALU.max), r=[sm, sink_tile], w=[sm])
        P.dve(lambda e: e.tensor_single_scalar(out=sm[:, 5:6], in_=sm[:, 4:5], scalar=-1.0, op=ALU.mult), r=[sm], w=[sm])
    else:
        P.dve(lambda e: e.tensor_single_scalar(out=sm[:, 5:6], in_=sm[:, 4:5], scalar=-scale, op=ALU.mult), r=[sm], w=[sm])
    P.dve(lambda e: e.memset(sm[:, 6:10], 0.0), r=[sm], w=[sm])
    off = 0
    for i, (b, w) in enumerate(zip(banks, widths)):
        P.act(lambda e, i=i, b=b, w=w, off=off: e.activation(out=sc.Pe[:, off:off + w], in_=b[:, 0:w], func=AF.Exp, bias=sm[:, 5:6],
                                                             scale=scale, accum_out=sm[:, 6 + i:7 + i]), r=[b, sm], w=[sc.Pe, sm])
        off += w
    P.dve(lambda e: e.tensor_reduce(out=sm[:, 10:11], in_=sm[:, 6:6 + nb_], axis=AX.X, op=ALU.add), r=[sm], w=[sm])
    if sink is not None:
        P.dve(lambda e: e.tensor_tensor(out=sm[:, 12:13], in0=sink, in1=sm[:, 4:5], op=ALU.subtract), r=[sm, sink_tile], w=[sm])
        P.act(lambda e: e.activation(out=sm[:, 12:13], in_=sm[:, 12:13], func=AF.Exp), r=[sm], w=[sm])
        P.dve(lambda e: e.tensor_tensor(out=sm[:, 10:11], in0=sm[:, 10:11], in1=sm[:, 12:13], op=ALU.add), r=[sm], w=[sm])
    P.dve(lambda e: e.reciprocal(out=sm[:, 11:12], in_=sm[:, 10:11]), r=[sm], w=[sm])


def transpose_blocks(k, src_tile, src_aps, dst_tile, dst_ap_fn, evac="act"):
    P = k.P
    n = len(src_aps)
    j0 = 0
    gi = 0
    while j0 < n:
        cnt = min(8, n - j0)
        pb = k.psbf()
        for j in range(cnt):
            P.pe(lambda e, j=j, j0=j0, pb=pb: e.transpose(pb[:, j * 128:(j + 1) * 128], src_aps[j0 + j], k.identb[:]),
                 r=[src_tile, k.identb], w=[pb])
        dst = dst_ap_fn(j0, cnt)
        srcv = pb[:, 0:cnt * 128].rearrange("p (n c) -> p n c", c=128)
        eng = evac if isinstance(evac, str) else evac[gi % len(evac)]
        if eng == "act":
            P.act(lambda e, dst=dst, srcv=srcv: e.copy(out=dst, in_=srcv), r=[pb], w=[dst_tile])
        else:
            P.dve(lambda e, dst=dst, srcv=srcv: e.tensor_copy(out=dst, in_=srcv), r=[pb], w=[dst_tile])
        j0 += cnt
        gi += 1


def rope_tm(k, src_tile, src, dst_tile, dst, tab, nh, R, tmp):
    P = k.P
    q = R // 4
    t1 = tmp[0].ap[:, 0:nh * R].rearrange("p (h r) -> p h r", r=R)
    t2 = tmp[1].ap[:, 0:nh * R].rearrange("p (h r) -> p h r", r=R)
    cosb = tab[:, 0, :].unsqueeze(1).to_broadcast([128, nh, R])
    P.dve(lambda e: e.tensor_tensor(out=t1, in0=src, in1=cosb, op=ALU.mult), r=[src_tile, k.rp], w=[tmp[0]])
    s5 = src.rearrange("p h (a b c) -> p h a b c", a=2, b=2)
    t5 = t2.rearrange("p h (a b c) -> p h a b c", a=2, b=2)
    sn5 = tab[:, 1, :].rearrange("p (a b c) -> p a b c", a=2, b=2)
    for bsel in (0, 1):
        sinb = sn5[:, :, bsel, :].unsqueeze(1).to_broadcast([128, nh, 2, q])
        P.dve(lambda e, bsel=bsel, sinb=sinb: e.tensor_tensor(out=t5[:, :, :, bsel, :], in0=s5[:, :, :, 1 - bsel, :], in1=sinb, op=ALU.mult),
              r=[src_tile, k.rp], w=[tmp[1]])
    P.dve(lambda e: e.tensor_tensor(out=dst, in0=t1, in1=t2, op=ALU.add), r=[tmp[0], tmp[1]], w=[dst_tile])


def out_proj_acc(k, g, l, w_dram_rows, mixh):
    P = k.P
    wo, ds = wslot(k)
    wov = wo.ap[:, 0:2048]
    P.dma("pool", ds, wov, w_dram_rows, w=[wo])
    for oc in range(16):
        for tb in range(g.ntb):
            ps = k.ps()
            P.pe(lambda e, ps=ps, oc=oc, tb=tb: e.matmul(ps[:], lhsT=wov[:, oc * 128:(oc + 1) * 128], rhs=mixh[:, tb * 512:(tb + 1) * 512],
                                                        start=True, stop=True), r=[wo, mixh], w=[ps])
            xt = k.xt[oc][tb]
            P.dve(lambda e, ps=ps, xt=xt, oc=oc: e.scalar_tensor_tensor(out=xt.ap, in0=ps[:], scalar=mod(k, l, 2, oc, g.vidx), in1=xt.ap,
                                                                       op0=ALU.mult, op1=ALU.add), r=[ps, xt, k.modv], w=[xt])


def make_hT(k, g, l, es, prescaled=False):
    P = k.P
    hT = P.sb(es, [128, 16, g.T], BF16, "hT")
    hTt = [Tl(hT[:, kk, :], "hT") for kk in range(16)]
    for kk in range(16):
        for tb in range(g.ntb):
            xt = k.xt[kk][tb]
            scl = mod(k, l, 6, kk, g.vidx) if prescaled else mod(k, l, 1, kk, g.vidx)
            P.act(lambda e, kk=kk, tb=tb, xt=xt, scl=scl: e.activation(out=hT[:, kk, tb * 512:(tb + 1) * 512], in_=xt.ap, func=AF.Identity,
                                                                      bias=mod(k, l, 0, kk, g.vidx), scale=scl),
                  r=[xt, k.modv], w=[hTt[kk]])
            if not prescaled:
                P.pool(lambda e, xt=xt: e.tensor_single_scalar(out=xt.ap, in_=xt.ap, scalar=ALPHA, op=ALU.mult), r=[xt], w=[xt])
    return hT, hTt


def proj_tm(k, hT, hTt, tt, wv, wt, n):
    P = k.P
    ps = k.ps()
    for kk in range(16):
        P.pe(lambda e, kk=kk, ps=ps: e.matmul(ps[:, 0:n], lhsT=hT[:, kk, tt * 128:(tt + 1) * 128], rhs=wv[:, kk],
                                             start=(kk == 0), stop=(kk == 15)), r=[hTt[kk], wt], w=[ps])
    return ps


def rms_rstd(k, ps_ap, ps_tile, n, eps, junk, ss):
    P = k.P
    P.dve(lambda e: e.memset(ss[:, 0:1], 0.0), r=[ss], w=[ss])
    P.act(lambda e: e.activation(out=junk[:, 0:n], in_=ps_ap, func=AF.Square, accum_out=ss[:, 0:1]), r=[ps_tile, ss], w=[junk, ss])
    P.act(lambda e: e.activation(out=ss[:, 1:2], in_=ss[:, 0:1], func=AF.Sqrt, bias=eps, scale=1.0 / n), r=[ss], w=[ss])
    P.dve(lambda e: e.reciprocal(out=ss[:, 1:2], in_=ss[:, 1:2]), r=[ss], w=[ss])


class HeadBufs:
    def __init__(self, k, g, es, nkeys, nscr):
        P = k.P
        self.nkt = nkeys // 128
        self.scr = [Scr(P, es, nkeys) for _ in range(nscr)]
        self.PTs = [P.tile(es, [128, self.nkt, 128], BF16, "PTs") for _ in range(2 if not g.smp else 1)]
        self.obs = [P.tile(es, [128, 128], BF16, "ob") for _ in range(2)]
        self.mixh = [P.tile(es, [128, g.T], BF16, "mixh") for _ in range(2 if not g.smp else 1)]
        self.un = 0


def pv_and_store(k, hb, sc, vt, kt0, nkt, mh, t0, post):
    P = k.P
    u = hb.un
    hb.un += 1
    pt = hb.PTs[u % len(hb.PTs)]
    transpose_blocks(k, sc.Pe, [sc.Pe[:, j * 128:(j + 1) * 128] for j in range(nkt)], pt,
                     lambda j0, c: pt[:, j0:j0 + c, :], evac=("act", "dve"))
    po = k.ps()
    for kt in range(nkt):
        P.pe(lambda e, kt=kt, po=po: e.matmul(po[:, 0:128], lhsT=pt[:, kt, :], rhs=vt[:, kt0 + kt, :], start=(kt == 0), stop=(kt == nkt - 1)),
             r=[pt, vt], w=[po])
    ob = hb.obs[u % 2]
    post(po, ob)
    transpose_blocks(k, ob, [ob[:, 0:128]], mh, lambda j0, c: mh[:, t0:t0 + 128].unsqueeze(1), evac="dve")


def even_mixer(k, g, l):
    import math
    P, dr = k.P, k.dr
    i = l // 2
    T, v, ntt = g.T, g.vidx, g.ntt
    NK = g.nseq * g.nk
    nkt = g.nk // 128
    lam_init = 0.8 - 0.6 * math.exp(-0.3 * l)
    sc_mla = (128 + 64) ** -0.5
    sc_diff = 64 ** -0.5
    nbuf = 1 if g.smp else 2
    with ExitStack() as es:
        bv = P.tile(es, [128, 1408], F32, "bv")
        P.dma("sp", k.cds, bv[:], dr["bvec_ev"][:, i, :], w=[bv])
        if g.smp:
            k.rp = P.tile(es, [128, 8, 2, 64], F32, "rp")
            P.dma("sp", k.mds[0], k.rp[:], dr["rope64"][:, :, :, :], w=[k.rp])
        else:
            k.rp = bv
        lamt = P.tile(es, [128, 8], F32, "lamt")
        lam4 = bv.ap[:, 1152:1408].rearrange("p (a b d) -> p a b d", a=2, b=2)
        lt = P.tile(es, [128, 2, 64], F32, "lt")
        P.dve(lambda e: e.tensor_tensor(out=lt[:], in0=lam4[:, :, 0, :], in1=lam4[:, :, 1, :], op=ALU.mult), r=[bv], w=[lt])
        P.dve(lambda e: e.tensor_reduce(out=lamt[:, 0:2], in_=lt[:], axis=AX.X, op=ALU.add), r=[lt], w=[lamt])
        P.act(lambda e: e.activation(out=lamt[:, 0:2], in_=lamt[:, 0:2], func=AF.Exp), r=[lamt], w=[lamt])
        P.dve(lambda e: e.tensor_scalar(out=lamt[:, 2:3], in0=lamt[:, 1:2], scalar1=lamt[:, 0:1], scalar2=-lam_init,
                                        op0=ALU.subtract, op1=ALU.add), r=[lamt], w=[lamt])
        sl2 = P.tile(es, [128, 128], F32, "sl2")
        P.dve(lambda e: e.tensor_single_scalar(out=sl2[:], in_=bv[:, 1024:1152], scalar=(1.0 - lam_init), op=ALU.mult), r=[bv], w=[sl2])
        junk = P.tile(es, [128, 512], F32, "junk")
        ss = P.tile(es, [128, 4], F32, "ss")
        rt = [P.tile(es, [128, 512], F32, "ropetmp") for _ in range(2)]
        tmb = [P.tile(es, [128, 512], BF16, "tmb") for _ in range(2)]
        tmf = P.tile(es, [128, 512], F32, "tmf")
        if not g.smp:
            stg_ckv = P.tile(es, [128, ntt, 512], F32, "stg_ckv")
            stg_kpe = P.tile(es, [128, ntt, 64], F32, "stg_kpe")
            stg_dk = P.tile(es, [128, ntt, 1024], F32, "stg_dk")
            stg_dv = P.tile(es, [128, ntt, 1024], F32, "stg_dv")
        qlnT = P.tile(es, [128, 4, T], BF16, "qlnT")
        ckvT = P.tile(es, [128, 4, NK], BF16, "ckvT")
        kpeT2 = P.tile(es, [128, NK], BF16, "kpeT2")
        qrT = P.tile(es, [128, 4, T], BF16, "qrT")
        hb = HeadBufs(k, g, es, g.nk, 2 if g.smp else 4)
        nq = 0
        with ExitStack() as es1:
            hT, hTt = make_hT(k, g, l, es1)
            es1a = ExitStack()
            alloc_wslots(k, es1a, 2)
            if g.smp:
                P.dma("pool", k.mds[3], ckvT[:, :, 1024:1536], dr["ckv_ctxT"][i].rearrange("(k p) t -> p k t", p=128), w=[ckvT])
                P.dma("pool", k.mds[4], kpeT2[:, 1024:1536], dr["kpe_ctxT2"][i], w=[kpeT2])
            for blk, (c0, n) in enumerate(((0, 512), (512, 512), (1024, 64))):
                wt, ds = wslot(k)
                wv = wt.ap[:, 0:16 * n].rearrange("p (k c) -> p k c", k=16)
                P.dma("pool", ds, wv, dr["ev_w_in"][i, :, c0:c0 + n].rearrange("(k p) c -> p k c", p=128), w=[wt])
                for tt in range(ntt):
                    ps = proj_tm(k, hT, hTt, tt, wv, wt, n)
                    tb_ = tmb[nq % 2]
                    nq += 1
                    if blk == 0:
                        rms_rstd(k, ps[:, 0:512], ps, 512, RMS_EPS, junk, ss)
                        P.dve(lambda e, ps=ps, tb_=tb_: e.scalar_tensor_tensor(out=tb_[:], in0=ps[:], scalar=ss[:, 1:2], in1=bv[:, 0:512],
                                                                               op0=ALU.mult, op1=ALU.mult), r=[ps, ss, bv], w=[tb_])
                        transpose_blocks(k, tb_, [tb_[:, j * 128:(j + 1) * 128] for j in range(4)], qlnT,
                                         lambda j0, c, tt=tt: qlnT[:, j0:j0 + c, tt * 128:(tt + 1) * 128])
                    elif blk == 1:
                        rms_rstd(k, ps[:, 0:512], ps, 512, RMS_EPS, junk, ss)
                        dstf = stg_ckv[:, tt, :] if not g.smp else tmf[:]
                        dstt = stg_ckv if not g.smp else tmf
                        P.dve(lambda e, ps=ps, dstf=dstf: e.scalar_tensor_tensor(out=dstf, in0=ps[:], scalar=ss[:, 1:2], in1=bv[:, 512:1024],
                                                                                op0=ALU.mult, op1=ALU.mult), r=[ps, ss, bv], w=[dstt])
                        P.pool(lambda e, tb_=tb_, dstf=dstf: e.tensor_copy(out=tb_[:], in_=dstf), r=[dstt], w=[tb_])
                        transpose_blocks(k, tb_, [tb_[:, j * 128:(j + 1) * 128] for j in range(4)], ckvT,
                                         lambda j0, c, tt=tt: ckvT[:, j0:j0 + c, tt * 128:(tt + 1) * 128])
                    else:
                        if g.smp:
                            rope_tm(k, ps, ps[:, 0:64].rearrange("p (h r) -> p h r", h=1), tmf, tmf[:, 0:64].rearrange("p (h r) -> p h r", h=1),
                                    k.rp[:, tt], 1, 64, rt)
                            srcf, srct = tmf[:, 0:64], tmf
                        else:
                            P.act(lambda e, ps=ps, tt=tt: e.copy(out=stg_kpe[:, tt, :], in_=ps[:, 0:64]), r=[ps], w=[stg_kpe])
                            srcf, srct = stg_kpe[:, tt, :], stg_kpe
                        for hh in range(2):
                            P.pool(lambda e, hh=hh, tb_=tb_, srcf=srcf: e.tensor_copy(out=tb_[:, hh * 64:(hh + 1) * 64], in_=srcf), r=[srct], w=[tb_])
                        transpose_blocks(k, tb_, [tb_[:, 0:128]], kpeT2, lambda j0, c, tt=tt: kpeT2[:, tt * 128:(tt + 1) * 128].unsqueeze(1))
            P.barrier()
            es1a.close()
            alloc_wslots(k, es1, 2, 6144)
            dqT = [P.tile(es1, [128, T], BF16, "dqT") for _ in range(nbuf)]
            dkT = [P.tile(es1, [128, NK], BF16, "dkT") for _ in range(nbuf)]
            dvh = [P.tile(es1, [128, NK // 128, 128], BF16, "dvh") for _ in range(nbuf)]
            c2 = P.tile(es1, [128, 2], F32, "c2")
            for h in range(OPT.get("ndiff", 8)):
                dq_, dk_, dv_, mh = dqT[h % nbuf], dkT[h % nbuf], dvh[h % nbuf], hb.mixh[h % len(hb.mixh)]
                wt, ds = wslot(k)
                wv = wt.ap[:, 0:16 * 384].rearrange("p (k g c) -> p k g c", k=16, g=3)
                srcw = dr["ev_w_in"][i, :, 1088:4160].rearrange("(k p) (g c) -> p k g c", p=128, g=3)
                for gi in range(3):
                    P.dma("pool", ds, wv[:, :, gi, :], srcw[:, :, gi, h * 128:(h + 1) * 128], w=[wt])
                if g.smp:
                    P.dma("pool", k.mds[5 + (h % 2)], dk_[:, 1024:1536], dr["dk_ctxT"][i, h], w=[dk_])
                    P.dma("pool", k.mds[7 + (h % 2)], dv_[:, 8:12, :], dr["dv_ctx"][i][:, h * 128:(h + 1) * 128].rearrange("(t p) c -> p t c", p=128), w=[dv_])
                for tt in range(ntt):
                    ps = proj_tm(k, hT, hTt, tt, wv, wt, 384)
                    tb_ = tmb[nq % 2]
                    nq += 1
                    dsub = OPT.get("dsub", 9)
                    if dsub < 1:
                        continue
                    if g.smp:
                        rope_tm(k, ps, ps[:, 0:256].rearrange("p (h r) -> p h r", r=64), tb_, tb_[:, 0:256].rearrange("p (h r) -> p h r", r=64),
                                k.rp[:, tt], 4, 64, rt)
                        P.dve(lambda e, ps=ps, tt=tt: e.tensor_copy(out=dv_[:, tt, :], in_=ps[:, 256:384]), r=[ps], w=[dv_])
                    else:
                        var = OPT.get("dvar", "abc")
                        if "a" in var:
                            P.act(lambda e, ps=ps, tb_=tb_: e.copy(out=tb_[:, 0:128], in_=ps[:, 0:128]), r=[ps], w=[tb_])
                        if "b" in var:
                            P.act(lambda e, ps=ps, tt=tt: e.copy(out=stg_dk[:, tt, h * 128:(h + 1) * 128], in_=ps[:, 128:256]), r=[ps], w=[stg_dk])
                        if "c" in var:
                            P.act(lambda e, ps=ps, tt=tt: e.copy(out=stg_dv[:, tt, h * 128:(h + 1) * 128], in_=ps[:, 256:384]), r=[ps], w=[stg_dv])
                        if dsub < 2:
                            continue
                        P.pool(lambda e, tb_=tb_, tt=tt: e.tensor_copy(out=tb_[:, 128:256], in_=stg_dk[:, tt, h * 128:(h + 1) * 128]), r=[stg_dk], w=[tb_])
                        P.pool(lambda e, tt=tt: e.tensor_copy(out=dv_[:, tt, :], in_=stg_dv[:, tt, h * 128:(h + 1) * 128]), r=[stg_dv], w=[dv_])
                    if dsub < 3:
                        continue
                    transpose_blocks(k, tb_, [tb_[:, 0:128]], dq_, lambda j0, c, tt=tt: dq_[:, tt * 128:(tt + 1) * 128].unsqueeze(1))
                    if dsub < 4:
                        continue
                    transpose_blocks(k, tb_, [tb_[:, 128:256]], dk_, lambda j0, c, tt=tt: dk_[:, tt * 128:(tt + 1) * 128].unsqueeze(1), evac="dve")
                for s in range(g.nseq if OPT.get("dstop", 9) >= 2 else 0):
                    kbase = s * g.nk
                    for qt in range(g.Sq // 128):
                        t0 = s * g.Sq + qt * 128
                        if g.smp:
                            sca, scb = hb.scr[0], hb.scr[1]
                        else:
                            sca, scb = hb.scr[hb.un % 2], hb.scr[2 + hb.un % 2]
                        for comp, sc in ((0, sca), (1, scb)):
                            banks, widths = [], []
                            for kb in range((g.nk + 511) // 512):
                                w_ = min(512, g.nk - kb * 512)
                                ps = k.ps()
                                c0 = kbase + kb * 512
                                P.pe(lambda e, ps=ps, w_=w_, c0=c0, t0=t0, comp=comp: e.matmul(
                                    ps[:, 0:w_], lhsT=dq_[comp * 64:(comp + 1) * 64, t0:t0 + 128], rhs=dk_[comp * 64:(comp + 1) * 64, c0:c0 + w_],
                                    start=True, stop=True), r=[dq_, dk_], w=[ps])
                                banks.append(ps)
                                widths.append(w_)
                            softmax_core(k, sc, banks, widths, sc_diff)
                        if OPT.get("dstop", 9) < 3:
                            continue
                        P.dve(lambda e, scb=scb: e.tensor_tensor(out=c2[:, 0:1], in0=scb.sm[:, 11:12], in1=lamt[:, 2:3], op=ALU.mult), r=[scb.sm, lamt], w=[c2])
                        P.dve(lambda e, scb=scb: e.tensor_scalar(out=scb.Pe[:], in0=scb.Pe[:], scalar1=c2[:, 0:1], scalar2=None, op0=ALU.mult),
                              r=[scb.Pe, c2], w=[scb.Pe])
                        P.dve(lambda e, sca=sca, scb=scb: e.scalar_tensor_tensor(out=sca.Pe[:], in0=sca.Pe[:], scalar=sca.sm[:, 11:12], in1=scb.Pe[:],
                                                                                op0=ALU.mult, op1=ALU.add), r=[sca.Pe, sca.sm, scb.Pe], w=[sca.Pe])

                        def post(po, ob):
                            rms_rstd(k, po[:, 0:128], po, 128, RMS_EPS, junk, ss)
                            P.dve(lambda e: e.scalar_tensor_tensor(out=ob[:], in0=po[:, 0:128], scalar=ss[:, 1:2], in1=sl2[:], op0=ALU.mult, op1=ALU.mult),
                                  r=[po, ss, sl2], w=[ob])
                        if OPT.get("dstop", 9) < 4:
                            continue
                        pv_and_store(k, hb, sca, dv_, kbase // 128, nkt, mh, t0, post)
                if OPT.get("dstop", 9) >= 5:
                    out_proj_acc(k, g, l, dr["ev_w_out"][i, (8 + h) * 128:(9 + h) * 128, :], mh)
            P.barrier()
        with ExitStack() as es2:
            alloc_wslots(k, es2, 2, 2048)
            wq = P.tile(es2, [128, 4, 1536], BF16, "wq")
            wkv = P.tile(es2, [128, 4, 2048], BF16, "wkv")
            P.dma("pool", k.mds[1], wq[:], dr["mla_wq_b"][i].rearrange("(k p) c -> p k c", p=128), w=[wq])
            P.dma("pool", k.mds[2], wkv[:], dr["mla_wkv_b"][i].rearrange("(k p) c -> p k c", p=128), w=[wkv])
            wq_rope = wq.ap[:, :, :].rearrange("p k (h c) -> p k h c", c=192)[:, :, :, 128:192]
            for tt in range(ntt):
                ps = k.ps()
                for kq in range(4):
                    P.pe(lambda e, kq=kq, ps=ps, tt=tt: e.matmul(ps[:], lhsT=qlnT[:, kq, tt * 128:(tt + 1) * 128], rhs=wq_rope[:, kq],
                                                                start=(kq == 0), stop=(kq == 3)), r=[qlnT, wq], w=[ps])
                tb_ = tmb[nq % 2]
                nq += 1
                if g.smp:
                    rope_tm(k, ps, ps[:, 0:512].rearrange("p (h r) -> p h r", r=64), tb_, tb_[:, 0:512].rearrange("p (h r) -> p h r", r=64),
                            k.rp[:, tt], 8, 64, rt)
                else:
                    P.act(lambda e, ps=ps, tb_=tb_: e.copy(out=tb_[:], in_=ps[:]), r=[ps], w=[tb_])
                transpose_blocks(k, tb_, [tb_[:, j * 128:(j + 1) * 128] for j in range(4)], qrT,
                                 lambda j0, c, tt=tt: qrT[:, j0:j0 + c, tt * 128:(tt + 1) * 128])
            qnT = [P.tile(es2, [128, T], BF16, "qnT") for _ in range(2)]
            knT = [P.tile(es2, [128, NK], BF16, "knT") for _ in range(2)]
            vtm = [P.tile(es2, [128, NK // 128, 128], BF16, "vtm") for _ in range(2)]
            for h in range(OPT.get("nmla", 8)):
                qn_, kn_, vt_, mh = qnT[h % 2], knT[h % 2], vtm[h % 2], hb.mixh[h % len(hb.mixh)]
                for tb in range(g.ntb):
                    ps = k.ps()
                    for kq in range(4):
                        P.pe(lambda e, kq=kq, ps=ps, tb=tb: e.matmul(ps[:], lhsT=wq[:, kq, h * 192:h * 192 + 128], rhs=qlnT[:, kq, tb * 512:(tb + 1) * 512],
                                                                    start=(kq == 0), stop=(kq == 3)), r=[wq, qlnT], w=[ps])
                    P.act(lambda e, ps=ps, tb=tb: e.copy(out=qn_[:, tb * 512:(tb + 1) * 512], in_=ps[:]), r=[ps], w=[qn_])
                for kb in range(NK // 512):
                    ps = k.ps()
                    for kq in range(4):
                        P.pe(lambda e, kq=kq, ps=ps, kb=kb: e.matmul(ps[:], lhsT=wkv[:, kq, h * 256:h * 256 + 128], rhs=ckvT[:, kq, kb * 512:(kb + 1) * 512],
                                                                    start=(kq == 0), stop=(kq == 3)), r=[wkv, ckvT], w=[ps])
                    P.dve(lambda e, ps=ps, kb=kb: e.tensor_copy(out=kn_[:, kb * 512:(kb + 1) * 512], in_=ps[:]), r=[ps], w=[kn_])
                for k4 in range(NK // 512):
                    ps = k.ps()
                    for j in range(4):
                        kt = k4 * 4 + j
                        for kq in range(4):
                            P.pe(lambda e, kq=kq, ps=ps, kt=kt, j=j: e.matmul(ps[:, j * 128:(j + 1) * 128], lhsT=ckvT[:, kq, kt * 128:(kt + 1) * 128],
                                                                             rhs=wkv[:, kq, h * 256 + 128:h * 256 + 256], start=(kq == 0), stop=(kq == 3)),
                                 r=[wkv, ckvT], w=[ps])
                    P.act(lambda e, ps=ps, k4=k4: e.copy(out=vt_[:, k4 * 4:(k4 + 1) * 4, :], in_=ps[:].rearrange("p (j c) -> p j c", c=128)), r=[ps], w=[vt_])
                hbp = (h % 2) * 64
                for s in range(g.nseq):
                    kbase = s * g.nk
                    for qt in range(g.Sq // 128):
                        t0 = s * g.Sq + qt * 128
                        sc = hb.scr[hb.un % 2]
                        banks, widths = [], []
                        for kb in range((g.nk + 511) // 512):
                            w_ = min(512, g.nk - kb * 512)
                            ps = k.ps()
                            c0 = kbase + kb * 512
                            P.pe(lambda e, ps=ps, w_=w_, c0=c0, t0=t0: e.matmul(ps[:, 0:w_], lhsT=qn_[:, t0:t0 + 128], rhs=kn_[:, c0:c0 + w_], start=True, stop=False),
                                 r=[qn_, kn_], w=[ps])
                            P.pe(lambda e, ps=ps, w_=w_, c0=c0, t0=t0: e.matmul(ps[:, 0:w_], lhsT=qrT[hbp:hbp + 64, h // 2, t0:t0 + 128], rhs=kpeT2[hbp:hbp + 64, c0:c0 + w_],
                                                                               start=False, stop=True), r=[qrT, kpeT2], w=[ps])
                            banks.append(ps)
                            widths.append(w_)
                        softmax_core(k, sc, banks, widths, sc_mla)

                        def post(po, ob, sc=sc):
                            P.act(lambda e: e.activation(out=ob[:], in_=po[:, 0:128], func=AF.Copy, scale=sc.sm[:, 11:12]), r=[po, sc.sm], w=[ob])
                        pv_and_store(k, hb, sc, vt_, kbase // 128, nkt, mh, t0, post)
                out_proj_acc(k, g, l, dr["ev_w_out"][i, h * 128:(h + 1) * 128, :], mh)
            P.barrier()
        if not g.smp:
            for s in range(2):
                for nm, st in (("nckv", stg_ckv), ("nkpe", stg_kpe), ("ndk", stg_dk), ("ndv", stg_dv)):
                    P.dma("sp", k.iods[2], dr[nm][s, i].rearrange("(t p) c -> p t c", p=128), st[:, 2 * s:2 * s + 2, :], r=[st])
        P.barrier()


def pv_store2(k, hb, sc, v_aps, v_tile, mh, t0, post):
    P = k.P
    nkt = len(v_aps)
    u = hb.un
    hb.un += 1
    pt = hb.PTs[u % len(hb.PTs)]
    transpose_blocks(k, sc.Pe, [sc.Pe[:, j * 128:(j + 1) * 128] for j in range(nkt)], pt,
                     lambda j0, c: pt[:, j0:j0 + c, :], evac=("act", "dve"))
    po = k.ps()
    for kt in range(nkt):
        P.pe(lambda e, kt=kt, po=po: e.matmul(po[:, 0:128], lhsT=pt[:, kt, :], rhs=v_aps[kt], start=(kt == 0), stop=(kt == nkt - 1)),
             r=[pt, v_tile], w=[po])
    ob = hb.obs[u % 2]
    post(po, ob)
    transpose_blocks(k, ob, [ob[:, 0:128]], mh, lambda j0, c: mh[:, t0:t0 + 128].unsqueeze(1), evac="dve")


def odd_mixer(k, g, l):
    P, dr = k.P, k.dr
    i = l // 2
    T, v, ntt = g.T, g.vidx, g.ntt
    NK = g.nseq * g.nk
    scale = 128 ** -0.5
    cps = g.Sq // 128
    with ExitStack() as es:
        bo = P.tile(es, [128, 1112], F32, "bo")
        P.dma("sp", k.cds, bo[:], dr["bvec_od"][:, i, :], w=[bo])
        tri = P.tile(es, [128, 4, 128], F32, "tri")
        P.dma("sp", k.mds[0], tri[:], dr["tri"][:, :, :], w=[tri])
        junk = P.tile(es, [128, 512], F32, "junk")
        ss = P.tile(es, [128, 4], F32, "ss")
        tmb = [P.tile(es, [128, 512], BF16, "tmb") for _ in range(2)]
        nq = 0
        xs_tm = P.tile(es, [128, ntt, 1024], BF16, "xs_tm")
        zs = P.tile(es, [128, ntt, 1024], BF16, "zs")
        BT = P.tile(es, [128, 2, T], BF16, "BT")
        CT = P.tile(es, [128, 2, T], BF16, "CT")
        B_tm = P.tile(es, [128, ntt, 256], BF16, "B_tm")
        dt_ = P.tile(es, [128, ntt, 32], F32, "dt")
        dta = P.tile(es, [128, ntt, 32], F32, "dta")
        abc = P.tile(es, [128, 32], F32, "abc")
        P.act(lambda e: e.activation(out=abc[:], in_=bo[:, 1056:1088], func=AF.Exp), r=[bo], w=[abc])
        P.dve(lambda e: e.tensor_single_scalar(out=abc[:], in_=abc[:], scalar=-1.0, op=ALU.mult), r=[abc], w=[abc])
        esh = ExitStack()
        hT, hTt = make_hT(k, g, l, esh)
        with ExitStack() as esb:
            alloc_wslots(k, esb, 2, 4096)
            cp = P.tile(esb, [128, 12, 4], F32, "convp")
            P.dma("sp", k.hds[1], cp[:], dr["convp"][:, i, :, :], w=[cp])
            W2 = g.nseq * (g.Sq + 2)
            raws = [P.tile(esb, [128, W2], F32, "raw") for _ in range(2)]
            for r_ in raws:
                P.pool(lambda e, r_=r_: e.memset(r_[:], 0.0), w=[r_])
            acc = P.tile(esb, [128, T], F32, "cacc")
            actb = [P.tile(esb, [128, T], BF16, "actb") for _ in range(2)]
            for b in range(6):
                wt, ds = wslot(k)
                wv = wt.ap[:, 0:4096].rearrange("p (k c) -> p k c", k=16)
                c0 = 2560 + b * 256
                P.dma("pool", ds, wv, dr["od_w_in"][i, :, c0:c0 + 256].rearrange("(k p) c -> p k c", p=128), w=[wt])
                for cc in range(2):
                    c = 2 * b + cc
                    raw = raws[c % 2]
                    raw3 = raw.ap[:, :].rearrange("p (s t) -> p s t", s=g.nseq)
                    for tb in range(g.ntb):
                        ps = k.ps()
                        for kk in range(16):
                            P.pe(lambda e, kk=kk, ps=ps, tb=tb, cc=cc, wv=wv: e.matmul(ps[:], lhsT=wv[:, kk, cc * 128:(cc + 1) * 128], rhs=hT[:, kk, tb * 512:(tb + 1) * 512],
                                                                                      start=(kk == 0), stop=(kk == 15)), r=[wt, hTt[kk]], w=[ps])
                        if g.smp:
                            P.act(lambda e, ps=ps, tb=tb, raw3=raw3: e.copy(out=raw3[:, 0, 1 + tb * 512:1 + (tb + 1) * 512], in_=ps[:]), r=[ps], w=[raw])
                        else:
                            P.act(lambda e, ps=ps, raw3=raw3: e.copy(out=raw3[:, :, 1:257], in_=ps[:].rearrange("p (s t) -> p s t", s=2)), r=[ps], w=[raw])
                    acc3 = acc.ap[:, :].rearrange("p (s t) -> p s t", s=g.nseq)
                    Sq = g.Sq
                    P.dve(lambda e, raw3=raw3, c=c: e.tensor_scalar(out=acc3, in0=raw3[:, :, 0:Sq], scalar1=cp[:, c, 0:1], scalar2=None, op0=ALU.mult),
                          r=[raw, cp], w=[acc])
                    for tap in (1, 2):
                        P.dve(lambda e, raw3=raw3, c=c, tap=tap: e.scalar_tensor_tensor(out=acc3, in0=raw3[:, :, tap:tap + Sq], scalar=cp[:, c, tap:tap + 1], in1=acc3,
                                                                                       op0=ALU.mult, op1=ALU.add), r=[raw, cp, acc], w=[acc])
                    ab = actb[c % 2]
                    P.act(lambda e, ab=ab, c=c: e.activation(out=ab[:], in_=acc[:], func=AF.Silu, bias=cp[:, c, 3:4], scale=1.0), r=[acc, cp], w=[ab])
                    if c < 8:
                        transpose_blocks(k, ab, [ab[:, t_ * 128:(t_ + 1) * 128] for t_ in range(ntt)], xs_tm,
                                         lambda j0, cnt, c=c: xs_tm[:, j0:j0 + cnt, c * 128:(c + 1) * 128], evac=("act", "dve"))
                    elif c < 10:
                        P.pool(lambda e, ab=ab, c=c: e.tensor_copy(out=BT[:, c - 8, :], in_=ab[:]), r=[ab], w=[BT])
                        transpose_blocks(k, ab, [ab[:, t_ * 128:(t_ + 1) * 128] for t_ in range(ntt)], B_tm,
                                         lambda j0, cnt, c=c: B_tm[:, j0:j0 + cnt, (c - 8) * 128:(c - 7) * 128], evac=("act", "dve"))
                    else:
                        P.pool(lambda e, ab=ab, c=c: e.tensor_copy(out=CT[:, c - 10, :], in_=ab[:]), r=[ab], w=[CT])
            for b in range(4):
                wt, ds = wslot(k)
                wv = wt.ap[:, 0:4096].rearrange("p (k c) -> p k c", k=16)
                c0 = 1536 + b * 256
                P.dma("pool", ds, wv, dr["od_w_in"][i, :, c0:c0 + 256].rearrange("(k p) c -> p k c", p=128), w=[wt])
                for tt in range(ntt):
                    ps = proj_tm(k, hT, hTt, tt, wv, wt, 256)
                    P.act(lambda e, ps=ps, tt=tt, b=b: e.activation(out=zs[:, tt, b * 256:(b + 1) * 256], in_=ps[:, 0:256], func=AF.Silu), r=[ps], w=[zs])
            wt, ds = wslot(k)
            wv = wt.ap[:, 0:512].rearrange("p (k c) -> p k c", k=16)
            P.dma("pool", ds, wv, dr["od_w_in"][i, :, 4096:4128].rearrange("(k p) c -> p k c", p=128), w=[wt])
            for tt in range(ntt):
                ps = proj_tm(k, hT, hTt, tt, wv, wt, 32)
                P.dve(lambda e, ps=ps, tt=tt: e.tensor_tensor(out=dt_[:, tt, :], in0=ps[:, 0:32], in1=bo[:, 1024:1056], op=ALU.add), r=[ps, bo], w=[dt_])
            QC = [0.9999999953848525, -0.4999990399276396, 0.3333000403522652, -0.2495455887119289, 0.19678117257163577,
                  -0.15311863105130397, 0.10614264692822836, -0.057064200825261056, 0.019907160963218935, -0.003256378481787808]
            spx = P.tile(esb, [128, 3, ntt * 32], F32, "spx")
            x0 = dt_.ap[:, :, :].rearrange("p t c -> p (t c)")
            ax, ex, rr = spx.ap[:, 0, :], spx.ap[:, 1, :], spx.ap[:, 2, :]
            SX = [spx]
            P.act(lambda e: e.activation(out=ax, in_=x0, func=AF.Abs), r=[dt_], w=SX)
            P.act(lambda e: e.activation(out=ex, in_=ax, func=AF.Exp, scale=-1.0), r=SX, w=SX)
            P.dve(lambda e: e.tensor_scalar(out=rr, in0=ex, scalar1=QC[9], scalar2=QC[8], op0=ALU.mult, op1=ALU.add), r=SX, w=SX)
            for ci in range(7, -1, -1):
                P.dve(lambda e: e.tensor_tensor(out=rr, in0=rr, in1=ex, op=ALU.mult), r=SX, w=SX)
                P.dve(lambda e, ci=ci: e.tensor_scalar_add(out=rr, in0=rr, scalar1=QC[ci]), r=SX, w=SX)
            P.dve(lambda e: e.tensor_tensor(out=rr, in0=rr, in1=ex, op=ALU.mult), r=SX, w=SX)
            P.dve(lambda e: e.tensor_single_scalar(out=ax, in_=x0, scalar=0.0, op=ALU.max), r=[dt_] + SX, w=SX)
            P.dve(lambda e: e.tensor_tensor(out=x0, in0=ax, in1=rr, op=ALU.add), r=SX, w=[dt_])
            P.dve(lambda e: e.tensor_tensor(out=dta[:, :, :], in0=dt_[:, :, :], in1=abc[:, :].unsqueeze(1).to_broadcast([128, ntt, 32]), op=ALU.mult),
                  r=[dt_, abc], w=[dta])
            P.barrier()
        if g.name == "P":
            dbg(k, "dt", dt_); dbg(k, "dta", dta); dbg(k, "xs", xs_tm); dbg(k, "Btm", B_tm); dbg(k, "BT", BT); dbg(k, "CT", CT); dbg(k, "zs", zs)
        with ExitStack() as esa:
            alloc_wslots(k, esa, 1 if g.smp else 2, 4096)
            rt = [P.tile(esa, [128, 256], F32, "ropetmp") for _ in range(2)]
            if g.smp:
                k.rp = P.tile(esa, [128, 8, 2, 128], F32, "rp128")
                P.dma("sp", k.hds[0], k.rp[:], dr["rope128"][:, :, :, :], w=[k.rp])
                trib = P.tile(esa, [128, 2, 128], BF16, "trib")
                P.dma("pool", k.mds[2], trib[:], dr["tri"][:, 2:4, :], w=[trib])
            else:
                stg_gk = P.tile(esa, [128, ntt, 256], F32, "stg_gk")
                stg_gv = P.tile(esa, [128, ntt, 256], F32, "stg_gv")
            kT = P.tile(esa, [128, 2, NK], BF16, "kT")
            vtm = P.tile(esa, [128, NK // 128, 256], BF16, "vtm")
            if g.smp:
                for j in range(2):
                    P.dma("pool", k.mds[3 + j], kT[:, j, 1024:1536], dr["gk_ctxT"][i, j], w=[kT])
                P.dma("pool", k.mds[5], vtm[:, 8:12, :], dr["gv_ctx"][i].rearrange("(t p) c -> p t c", p=128), w=[vtm])
            for blk in range(2):
                wt, ds = wslot(k)
                wv = wt.ap[:, 0:4096].rearrange("p (k c) -> p k c", k=16)
                c0 = 1024 + blk * 256
                P.dma("pool", ds, wv, dr["od_w_in"][i, :, c0:c0 + 256].rearrange("(k p) c -> p k c", p=128), w=[wt])
                for tt in range(ntt):
                    ps = proj_tm(k, hT, hTt, tt, wv, wt, 256)
                    if blk == 0:
                        tb_ = tmb[nq % 2]
                        nq += 1
                        if g.smp:
                            rope_tm(k, ps, ps[:, 0:256].rearrange("p (h r) -> p h r", r=128), tb_, tb_[:, 0:256].rearrange("p (h r) -> p h r", r=128),
                                    k.rp[:, tt], 2, 128, rt)
                        else:
                            P.act(lambda e, ps=ps, tt=tt: e.copy(out=stg_gk[:, tt, :], in_=ps[:, 0:256]), r=[ps], w=[stg_gk])
                            P.pool(lambda e, tb_=tb_, tt=tt: e.tensor_copy(out=tb_[:, 0:256], in_=stg_gk[:, tt, :]), r=[stg_gk], w=[tb_])
                        transpose_blocks(k, tb_, [tb_[:, j * 128:(j + 1) * 128] for j in range(2)], kT,
                                         lambda j0, c, tt=tt: kT[:, j0:j0 + c, tt * 128:(tt + 1) * 128])
                    else:
                        if g.smp:
                            P.act(lambda e, ps=ps, tt=tt: e.copy(out=vtm[:, tt, :], in_=ps[:, 0:256]), r=[ps], w=[vtm])
                        else:
                            P.act(lambda e, ps=ps, tt=tt: e.copy(out=stg_gv[:, tt, :], in_=ps[:, 0:256]), r=[ps], w=[stg_gv])
                            P.pool(lambda e, tt=tt: e.tensor_copy(out=vtm[:, tt, :], in_=stg_gv[:, tt, :]), r=[stg_gv], w=[vtm])
            nkmax = 896 if g.smp else 256
            hb = HeadBufs(k, g, esa, nkmax, 2)
            qT = [P.tile(esa, [128, 2, T], BF16, "qT") for _ in range(2)]
            for qb in range(OPT.get("nqb", 4)):
                qT_ = qT[qb % 2]
                wt, ds = wslot(k)
                wv = wt.ap[:, 0:4096].rearrange("p (k c) -> p k c", k=16)
                P.dma("pool", ds, wv, dr["od_w_in"][i, :, qb * 256:(qb + 1) * 256].rearrange("(k p) c -> p k c", p=128), w=[wt])
                for tt in range(ntt):
                    ps = proj_tm(k, hT, hTt, tt, wv, wt, 256)
                    tb_ = tmb[nq % 2]
                    nq += 1
                    if g.smp:
                        rope_tm(k, ps, ps[:, 0:256].rearrange("p (h r) -> p h r", r=128), tb_, tb_[:, 0:256].rearrange("p (h r) -> p h r", r=128),
                                k.rp[:, tt], 2, 128, rt)
                    else:
                        P.act(lambda e, ps=ps, tb_=tb_: e.copy(out=tb_[:, 0:256], in_=ps[:, 0:256]), r=[ps], w=[tb_])
                    transpose_blocks(k, tb_, [tb_[:, j * 128:(j + 1) * 128] for j in range(2)], qT_,
                                     lambda j0, c, tt=tt: qT_[:, j0:j0 + c, tt * 128:(tt + 1) * 128])
                for hh in range(2):
                    h = qb * 2 + hh
                    j = h // 4
                    mh = hb.mixh[h % len(hb.mixh)]
                    sink = bo[:, 1104 + h:1105 + h]
                    for s in range(g.nseq):
                        for qt in range(cps):
                            t0 = s * g.Sq + qt * 128
                            sc = hb.scr[hb.un % 2]
                            if not g.smp:
                                ps = k.ps()
                                P.pe(lambda e, ps=ps, t0=t0, s=s: e.matmul(ps[:, 0:256], lhsT=qT_[:, hh, t0:t0 + 128], rhs=kT[:, j, s * 256:(s + 1) * 256],
                                                                          start=True, stop=True), r=[qT_, kT], w=[ps])
                                banks, widths = [ps], [256]
                                v_aps = [vtm[:, s * 2 + b, j * 128:(j + 1) * 128] for b in range(2)]
                            else:
                                lo, hi = max(qt - 1, 0), min(qt + 1, cps - 1)
                                wl = (hi - lo + 1) * 128
                                pa, pb_ = k.ps(), k.ps()
                                nmask = (1 if qt > 0 else 0) + (1 if qt < cps - 1 else 0)
                                P.pe(lambda e, pa=pa, t0=t0, lo=lo, hi=hi, wl=wl, nmask=nmask: e.matmul(
                                    pa[:, 0:wl], lhsT=qT_[:, hh, t0:t0 + 128], rhs=kT[:, j, lo * 128:(hi + 1) * 128], start=True, stop=(nmask == 0)),
                                    r=[qT_, kT], w=[pa])
                                if qt > 0:
                                    nmask -= 1
                                    P.pe(lambda e, pa=pa, nmask=nmask: e.matmul(pa[:, 0:128], lhsT=k.identb[:], rhs=trib[:, 0, :], start=False, stop=(nmask == 0)),
                                         r=[k.identb, trib], w=[pa])
                                if qt < cps - 1:
                                    P.pe(lambda e, pa=pa, wl=wl: e.matmul(pa[:, wl - 128:wl], lhsT=k.identb[:], rhs=trib[:, 1, :], start=False, stop=True),
                                         r=[k.identb, trib], w=[pa])
                                P.pe(lambda e, pb_=pb_, t0=t0: e.matmul(pb_[:, 0:512], lhsT=qT_[:, hh, t0:t0 + 128], rhs=kT[:, j, 1024:1536], start=True, stop=True),
                                     r=[qT_, kT], w=[pb_])
                                banks, widths = [pa, pb_], [wl, 512]
                                v_aps = [vtm[:, t_, j * 128:(j + 1) * 128] for t_ in range(lo, hi + 1)] + [vtm[:, 8 + t_, j * 128:(j + 1) * 128] for t_ in range(4)]
                            softmax_core(k, sc, banks, widths, scale, sink=sink, sink_tile=bo)

                            def post(po, ob, sc=sc):
                                P.act(lambda e: e.activation(out=ob[:], in_=po[:, 0:128], func=AF.Copy, scale=sc.sm[:, 11:12]), r=[po, sc.sm], w=[ob])
                            pv_store2(k, hb, sc, v_aps, vtm, mh, t0, post)
                    out_proj_acc(k, g, l, dr["od_w_out"][i, h * 128:(h + 1) * 128, :], mh)
            if not g.smp:
                for s in range(2):
                    for nm, st_ in (("ngk", stg_gk), ("ngv", stg_gv)):
                        P.dma("sp", k.iods[2], dr[nm][s, i].rearrange("(t p) c -> p t c", p=128), st_[:, 2 * s:2 * s + 2, :], r=[st_])
            P.barrier()
        esh.close()
        with ExitStack() as esc:
            alloc_wslots(k, esc, 2, 2048)
            y_acc = P.tile(esc, [128, ntt, 512], F32, "y_acc")
            st = P.tile(esc, [128, 512], F32, "st")
            stb = P.tile(esc, [128, 512], BF16, "stb")
            GT = P.tile(esc, [128, ntt, 128], F32, "GT")
            cs = P.tile(esc, [128, 48], F32, "cs")
            Ud = [P.tile(esc, [128, 128], F32, "Ud") for _ in range(2)]
            t1s = [P.tile(esc, [128, 128], F32, "t1") for _ in range(2)]
            decs = [P.tile(esc, [128, 128], F32, "dec") for _ in range(2)]
            MTs = [P.tile(esc, [128, 128], BF16, "MT") for _ in range(2)]
            xdt = P.tile(esc, [128, 512], BF16, "xdt")
            xw = P.tile(esc, [128, 512], BF16, "xw")
            ytmp = P.tile(esc, [128, 512], F32, "ytmp")
            mixc = [P.tile(esc, [128, T], BF16, "mixc") for _ in range(4)]
            if g.smp:
                h0t = P.tile(esc, [64, 8, 128], F32, "h0t")

            def v3(ap):
                return ap.rearrange("p (h c) -> p h c", c=64)

            def bc8(ap):
                return ap.unsqueeze(2).to_broadcast([128, 8, 64])
            psD = k.psrot.pop()
            for G in range(2):
                for tt in range(ntt):
                    pg = k.ps()
                    P.pe(lambda e, pg=pg, tt=tt: e.matmul(pg[:, 0:128], lhsT=BT[:, G, tt * 128:(tt + 1) * 128], rhs=CT[:, G, tt * 128:(tt + 1) * 128], start=True, stop=True),
                         r=[BT, CT], w=[pg])
                    P.act(lambda e, pg=pg, tt=tt: e.copy(out=GT[:, tt, :], in_=pg[:, 0:128]), r=[pg], w=[GT])
                    P.dve(lambda e, tt=tt: e.tensor_tensor(out=v3(y_acc[:, tt, :]), in0=v3(xs_tm[:, tt, G * 512:(G + 1) * 512]),
                                                            in1=bc8(bo[:, 1088 + G * 8:1096 + G * 8]), op=ALU.mult), r=[xs_tm, bo], w=[y_acc])
                for s in range(g.nseq):
                    for d in range(2):
                        col0 = d * 16 + G * 8
                        Utri = tri[:, d, :]
                        mask = tri[:, 2 + d, :]
                        if g.smp:
                            P.dma("sp", k.hds[2], h0t[:], dr["ssd_h0"][d, i, G * 8:(G + 1) * 8].rearrange("h p n -> p h n"), w=[h0t])
                            ph = k.ps()
                            for h in range(8):
                                P.pe(lambda e, ph=ph, h=h: e.transpose(ph[:, h * 64:(h + 1) * 64], h0t[:, h, :], k.identf[0:64, 0:64]), r=[h0t, k.identf], w=[ph])
                            P.act(lambda e, ph=ph: e.copy(out=st[:], in_=ph[:]), r=[ph], w=[st])
                        else:
                            P.pool(lambda e: e.memset(st[:], 0.0), w=[st])
                        P.pool(lambda e: e.tensor_copy(out=stb[:], in_=st[:]), r=[st], w=[stb])
                        order = list(range(s * cps, (s + 1) * cps))
                        if d == 1:
                            order = order[::-1]
                        for tt in order:
                            dcol = dta[:, tt, col0:col0 + 8]
                            psS = k.ps()
                            P.pe(lambda e, psS=psS, dcol=dcol: e.matmul(psS[:, 0:8], lhsT=Utri, rhs=dcol, start=True, stop=True), r=[tri, dta], w=[psS])
                            P.pe(lambda e, psS=psS, dcol=dcol: e.matmul(psS[:, 8:16], lhsT=k.onesf[:], rhs=dcol, start=True, stop=True), r=[k.onesf, dta], w=[psS])
                            CS = [cs]
                            P.act(lambda e, psS=psS: e.copy(out=cs[:, 0:16], in_=psS[:, 0:16]), r=[psS], w=CS)
                            P.dve(lambda e: e.tensor_single_scalar(out=cs[:, 16:24], in_=cs[:, 0:8], scalar=-1.0, op=ALU.mult), r=CS, w=CS)
                            P.act(lambda e: e.activation(out=cs[:, 24:32], in_=cs[:, 0:8], func=AF.Exp), r=CS, w=CS)
                            P.dve(lambda e: e.tensor_tensor(out=cs[:, 32:40], in0=cs[:, 8:16], in1=cs[:, 0:8], op=ALU.subtract), r=CS, w=CS)
                            P.act(lambda e: e.activation(out=cs[:, 32:40], in_=cs[:, 32:40], func=AF.Exp), r=CS, w=CS)
                            P.act(lambda e: e.activation(out=cs[:, 40:48], in_=cs[:, 8:16], func=AF.Exp), r=CS, w=CS)
                            xsv = v3(xs_tm[:, tt, G * 512:(G + 1) * 512])
                            P.dve(lambda e, tt=tt, xsv=xsv: e.tensor_tensor(out=v3(xdt[:, :]), in0=xsv, in1=bc8(dt_[:, tt, col0:col0 + 8]), op=ALU.mult),
                                  r=[xs_tm, dt_], w=[xdt])
                            P.dve(lambda e: e.tensor_tensor(out=v3(xw[:, :]), in0=v3(xdt[:, :]), in1=bc8(cs[:, 32:40]), op=ALU.mult), r=[xdt, cs], w=[xw])
                            for h in range(8):
                                ud, t1, dc, mt = Ud[h % 2], t1s[h % 2], decs[h % 2], MTs[h % 2]
                                P.dve(lambda e, ud=ud, tt=tt, h=h: e.tensor_scalar(out=ud[:], in0=Utri, scalar1=dta[:, tt, col0 + h:col0 + h + 1], scalar2=None, op0=ALU.mult),
                                      r=[tri, dta], w=[ud])
                                pc = k.ps()
                                P.pe(lambda e, pc=pc, ud=ud: e.matmul(pc[:, 0:128], lhsT=k.onesf[:], rhs=ud[:], start=True, stop=True), r=[k.onesf, ud], w=[pc])
                                P.dve(lambda e, pc=pc, t1=t1: e.tensor_tensor(out=t1[:], in0=pc[:, 0:128], in1=mask, op=ALU.add), r=[pc, tri], w=[t1])
                                P.act(lambda e, t1=t1, dc=dc, h=h: e.activation(out=dc[:], in_=t1[:], func=AF.Exp, bias=cs[:, 16 + h:17 + h], scale=1.0), r=[t1, cs], w=[dc])
                                P.pool(lambda e, mt=mt, dc=dc, tt=tt: e.tensor_tensor(out=mt[:], in0=GT[:, tt, :], in1=dc[:], op=ALU.mult), r=[GT, dc], w=[mt])
                                P.pe(lambda e, psD=psD, mt=mt, h=h: e.matmul(psD[:, h * 64:(h + 1) * 64], lhsT=mt[:], rhs=xdt[:, h * 64:(h + 1) * 64], start=True, stop=True),
                                     r=[mt, xdt], w=[psD])
                            psO = k.ps()
                            P.pe(lambda e, psO=psO, tt=tt: e.matmul(psO[:], lhsT=CT[:, G, tt * 128:(tt + 1) * 128], rhs=stb[:], start=True, stop=True), r=[CT, stb], w=[psO])
                            P.dve(lambda e, psD=psD, tt=tt: e.tensor_tensor(out=y_acc[:, tt, :], in0=psD[:], in1=y_acc[:, tt, :], op=ALU.add), r=[psD, y_acc], w=[y_acc])
                            P.dve(lambda e, psO=psO: e.tensor_tensor(out=v3(ytmp[:, :]), in0=v3(psO[:]), in1=bc8(cs[:, 24:32]), op=ALU.mult), r=[psO, cs], w=[ytmp])
                            P.pool(lambda e, tt=tt: e.tensor_tensor(out=y_acc[:, tt, :], in0=y_acc[:, tt, :], in1=ytmp[:], op=ALU.add), r=[ytmp, y_acc], w=[y_acc])
                            psT = k.ps()
                            P.pe(lambda e, psT=psT, tt=tt: e.matmul(psT[:], lhsT=B_tm[:, tt, G * 128:(G + 1) * 128], rhs=xw[:], start=True, stop=True), r=[B_tm, xw], w=[psT])
                            P.dve(lambda e: e.tensor_tensor(out=v3(st[:, :]), in0=v3(st[:, :]), in1=bc8(cs[:, 40:48]), op=ALU.mult), r=[st, cs], w=[st])
                            P.dve(lambda e, psT=psT: e.tensor_tensor(out=st[:], in0=psT[:], in1=st[:], op=ALU.add), r=[psT, st], w=[st])
                            P.pool(lambda e: e.tensor_copy(out=stb[:], in_=st[:]), r=[st], w=[stb])
                        if not g.smp:
                            P.dma("sp", k.hds[3], dr["nssd"][d, s, i, G * 8:(G + 1) * 8].rearrange("h n p -> n h p"), v3(st[:, :]), r=[st])
                if g.name == "P":
                    dbg(k, "yacc%d" % G, y_acc)
                for tt in range(ntt):
                    P.dve(lambda e, tt=tt: e.tensor_tensor(out=y_acc[:, tt, :], in0=y_acc[:, tt, :], in1=zs[:, tt, G * 512:(G + 1) * 512], op=ALU.mult),
                          r=[y_acc, zs], w=[y_acc])
                    P.dve(lambda e: e.memset(ss[:, 0:1], 0.0), r=[ss], w=[ss])
                    P.act(lambda e, tt=tt: e.activation(out=junk[:], in_=y_acc[:, tt, :], func=AF.Square, accum_out=ss[:, 0:1]), r=[y_acc, ss], w=[junk, ss])
                    P.act(lambda e: e.activation(out=ss[:, 1:2], in_=ss[:, 0:1], func=AF.Sqrt, bias=RMS_EPS, scale=1.0 / 512), r=[ss], w=[ss])
                    P.dve(lambda e: e.reciprocal(out=ss[:, 1:2], in_=ss[:, 1:2]), r=[ss], w=[ss])
                    tb_ = tmb[nq % 2]
                    nq += 1
                    P.dve(lambda e, tt=tt, tb_=tb_: e.scalar_tensor_tensor(out=tb_[:], in0=y_acc[:, tt, :], scalar=ss[:, 1:2], in1=bo[:, G * 512:(G + 1) * 512],
                                                                           op0=ALU.mult, op1=ALU.mult), r=[y_acc, ss, bo], w=[tb_])
                    for jj in range(4):
                        transpose_blocks(k, tb_, [tb_[:, jj * 128:(jj + 1) * 128]], mixc[jj],
                                         lambda j0, c, tt=tt, jj=jj: mixc[jj][:, tt * 128:(tt + 1) * 128].unsqueeze(1))
                for jj in range(4):
                    ch = 8 + G * 4 + jj
                    out_proj_acc(k, g, l, dr["od_w_out"][i, ch * 128:(ch + 1) * 128, :], mixc[jj])
            k.psrot.append(psD)
            P.barrier()


def _rope_tables():
    out = {}
    for R in (64, 128):
        half = R // 2
        quarter = half // 2
        S = 1024
        row = np.repeat(np.arange(S // 64), 64).astype(np.float32)
        col = (np.arange(S) % 64).astype(np.float32)
        inv = (10000.0 ** (-np.arange(quarter, dtype=np.float32) * 2.0 / half)).astype(np.float32)
        cos = np.zeros((S, R), np.float32)
        sin = np.zeros((S, R), np.float32)
        for base, pos in ((0, row), (half, col)):
            ang = (pos[:, None] * inv[None, :]).astype(np.float32)
            c, s = np.cos(ang), np.sin(ang)
            cos[:, base:base + quarter] = c
            cos[:, base + quarter:base + half] = c
            sin[:, base:base + quarter] = -s
            sin[:, base + quarter:base + half] = s
        tab = np.stack([cos, sin], axis=1)
        out[R] = np.ascontiguousarray(tab.reshape(8, 128, 2, R).transpose(1, 0, 2, 3))
    return out


def make_in_maps(inp, n_cores=8, depth=DEPTH):
    f = lambda a: np.ascontiguousarray(a, dtype=np.float32)
    rep = lambda v: np.broadcast_to(np.asarray(v, np.float32).reshape(1, -1), (128, np.asarray(v).size))
    fm = lambda v, nch: np.asarray(v, np.float32).reshape(nch, 128).T
    shared = {}
    shared["w_mod"] = f(inp["w_mod"][:depth])
    shared["b_modT"] = f(np.stack([fm(inp["b_mod"][l], 96) for l in range(4)], axis=1))
    shared["lnp"] = f(np.stack([np.stack([fm(inp[n][l], 16) for n in ("ln1_g", "ln1_b", "ln2_g", "ln2_b")], axis=1)
                                for l in range(4)], axis=1))
    for n in ("ev_w_in", "mla_wq_b", "mla_wkv_b", "ev_w_out", "od_w_in", "od_w_out"):
        shared[n] = f(inp[n])
    for n in ("moe_w_gate", "moe_w_up", "moe_w_down"):
        shared[n] = f(inp[n][:depth])
    shared["bvec_ev"] = f(np.stack([np.concatenate([rep(inp["mla_q_norm"][i]), rep(inp["mla_kv_norm"][i]),
                                                    rep(inp["diff_subln"][i]), rep(inp["diff_lambda"][i])], axis=1)
                                    for i in range(2)], axis=1))
    shared["bvec_od"] = f(np.stack([np.concatenate([rep(inp["ssd_norm"][i]), rep(inp["ssd_dt_bias"][i]), rep(inp["ssd_a_log"][i]),
                                                    rep(inp["ssd_d"][i]), rep(inp["gqa_sink"][i])], axis=1)
                                    for i in range(2)], axis=1))
    cw = np.concatenate([np.asarray(inp["ssd_conv_w"], np.float32), np.asarray(inp["ssd_conv_b"], np.float32)[:, None, :]], axis=1)
    shared["convp"] = f(cw.reshape(2, 4, 12, 128).transpose(3, 0, 2, 1))
    shared["moe_rt"] = f(np.concatenate([inp["moe_router_group"], inp["moe_router_expert"]], axis=2))
    sel = np.zeros((16, 16, 128), np.float32)
    for e in range(16):
        sel[e, e, :] = 1.0
    shared["selE"] = sel
    rt = _rope_tables()
    shared["rope64"], shared["rope128"] = rt[64], rt[128]
    tri = np.zeros((128, 4, 128), np.float32)
    ii = np.arange(128)
    tri[:, 0, :] = (ii[:, None] <= ii[None, :])
    tri[:, 1, :] = (ii[:, None] >= ii[None, :])
    tri[:, 2, :] = np.where(ii[:, None] <= ii[None, :], 0.0, -30000.0)
    tri[:, 3, :] = np.where(ii[:, None] >= ii[None, :], 0.0, -30000.0)
    shared["tri"] = tri
    maps = []
    for c in range(n_cores):
        b = c // 4
        m = dict(shared)
        m["xpT"] = f(np.asarray(inp["x_prompt"][2 * c:2 * c + 2]).reshape(512, D).T)
        m["xsT"] = f(np.asarray(inp["x_sample"][b]).T)
        cv = np.stack([np.asarray(inp["c_ctx"]), np.asarray(inp["c"][b])], axis=0)
        m["cT"] = f(cv.reshape(2, 16, 128).transpose(2, 1, 0))
        m["ckv_ctxT"] = f(np.asarray(inp["cache_mla_ckv"][b]).transpose(0, 2, 1))
        kp = np.asarray(inp["cache_mla_kpe"][b]).transpose(0, 2, 1)
        m["kpe_ctxT2"] = f(np.concatenate([kp, kp], axis=1))
        m["dk_ctxT"] = f(np.asarray(inp["cache_diff_k"][b]).transpose(0, 2, 3, 1))
        m["dv_ctx"] = f(np.asarray(inp["cache_diff_v"][b]).reshape(2, 512, 1024))
        m["gk_ctxT"] = f(np.asarray(inp["cache_gqa_k"][b]).transpose(0, 2, 3, 1))
        m["gv_ctx"] = f(np.asarray(inp["cache_gqa_v"][b]).reshape(2, 512, 256))
        m["ssd_h0"] = f(np.stack([np.asarray(inp["state_ssd_fwd"][b]), np.asarray(inp["state_ssd_bwd"][b])], axis=0))
        maps.append(m)
    return maps


def assemble(results):
    yp = np.concatenate([r["ypT"].T.reshape(2, 256, D) for r in results], axis=0)
    ys = np.stack([results[0]["ysT"].T, results[4]["ysT"].T], axis=0)
    cat = lambda n: np.concatenate([r[n] for r in results], axis=0)
    nckv, nkpe = cat("nckv"), cat("nkpe")
    ndk = cat("ndk").reshape(16, 2, 256, 8, 128)
    ndv = cat("ndv").reshape(16, 2, 256, 8, 128)
    ngk = cat("ngk").reshape(16, 2, 256, 2, 128)
    ngv = cat("ngv").reshape(16, 2, 256, 2, 128)
    ns = np.concatenate([r["nssd"] for r in results], axis=1)
    ns = ns.transpose(0, 1, 2, 3, 5, 4)
    out = (yp, ys, nckv, nkpe, ndk, ndv, ngk, ngv, ns[0], ns[1])
    return tuple(np.ascontiguousarray(o, dtype=np.float32) for o in out)


_CACHE = {}


def kernel(**inputs):
    if "nc" not in _CACHE:
        _CACHE["nc"] = build()[0]
    nc = _CACHE["nc"]
    maps = make_in_maps(inputs)
    res = run_bass_kernel_spmd(nc, maps, core_ids=list(range(8)))
    return assemble(res.results)
```
